# Optimizing a Trainium2 kernel written in Bass

```python
import jax, jax.numpy as jnp
from jax import lax
import numpy as np

D_MODEL = 2048
BATCH = 2
SEQ = 8192
DEPTH = 1

D_MIX = D_MODEL
CONV_WIDTH = D_MIX // 2
CONV_HEADS = 8
CONV_K = 3
POOL_WIDTH = D_MIX - CONV_WIDTH
POOL_WINDOWS = (2, 4, 8, 16)
POOL_GROUPS = len(POOL_WINDOWS)
POOL_GROUP_DIM = POOL_WIDTH // POOL_GROUPS
IN_PROJ_WIDTH = 3 * CONV_WIDTH + POOL_WIDTH
PEER_HEADS = 8
PEER_N_KEYS = 128
PEER_N_EXPERTS = PEER_N_KEYS * PEER_N_KEYS
PEER_TOPK = 16
PEER_D_KEY = 256
PEER_HALF = PEER_D_KEY // 2
PEER_CHUNK = 128
RMS_EPS = 1e-6

kernel_name = "hybrid_conv_pool_peer_block"


def rmsnorm(x, g):
    xf = x.astype(jnp.float32)
    y = xf * lax.rsqrt(jnp.mean(xf * xf, axis=-1, keepdims=True) + RMS_EPS)
    return (y * g.astype(jnp.float32)).astype(x.dtype)


def causal_short_conv(z, w, b):
    seq = z.shape[1]
    zp = jnp.pad(z, ((0, 0), (CONV_K - 1, 0), (0, 0)))
    y = b
    for k in range(CONV_K):
        y = y + w[k] * zp[:, k:k + seq, :]
    return y


def causal_multiscale_pool(z):
    bsz, seq, _ = z.shape
    zg = z.astype(jnp.float32).reshape(bsz, seq, POOL_GROUPS, POOL_GROUP_DIM)
    cs0 = jnp.concatenate([jnp.zeros((bsz, 1, POOL_GROUPS, POOL_GROUP_DIM), jnp.float32),
                           jnp.cumsum(zg, axis=1)], axis=1)
    pos = jnp.arange(1, seq + 1, dtype=jnp.float32)
    outs = []
    for g, w in enumerate(POOL_WINDOWS):
        c = cs0[:, :, g, :]
        upper = c[:, 1:, :]
        lower = jnp.concatenate([jnp.zeros((bsz, w - 1, POOL_GROUP_DIM), jnp.float32),
                                 c[:, :seq - w + 1, :]], axis=1)
        count = jnp.minimum(pos, float(w))[None, :, None]
        outs.append((upper - lower) / count - zg[:, :, g, :])
    return jnp.stack(outs, axis=2).astype(z.dtype)


def peer_ffn(h, w_q, sub_keys, expert_u, expert_v):
    bsz, seq, d = h.shape
    t = bsz * seq
    ht = h.reshape(t, d)
    q = (ht @ w_q).reshape(t, PEER_HEADS, 2, PEER_HALF).astype(jnp.float32)
    scores = jnp.einsum('thpd,hpkd->thpk', q, sub_keys.astype(jnp.float32))
    sv, si = lax.top_k(scores, PEER_TOPK)
    cand = (sv[:, :, 0, :, None] + sv[:, :, 1, None, :]).reshape(t, PEER_HEADS, PEER_TOPK * PEER_TOPK)
    cand_idx = (si[:, :, 0, :, None] * PEER_N_KEYS + si[:, :, 1, None, :]).reshape(t, PEER_HEADS, PEER_TOPK * PEER_TOPK)
    top_s, top_pos = lax.top_k(cand, PEER_TOPK)
    eidx = jnp.take_along_axis(cand_idx, top_pos, axis=-1)
    gates = jax.nn.softmax(top_s, axis=-1)
    n_chunks = t // PEER_CHUNK
    hk = PEER_HEADS * PEER_TOPK
    xs = ht.reshape(n_chunks, PEER_CHUNK, d)
    es = eidx.reshape(n_chunks, PEER_CHUNK, hk)
    gs = gates.astype(h.dtype).reshape(n_chunks, PEER_CHUNK, hk)

    def expert_chunk(args):
        xc, ec, gc = args
        u = jnp.take(expert_u, ec, axis=0)
        act = jax.nn.gelu(jnp.einsum('cd,ckd->ck', xc, u))
        v = jnp.take(expert_v, ec, axis=0)
        return jnp.einsum('ck,ckd->cd', gc * act, v)

    out = lax.map(expert_chunk, (xs, es, gs))
    return out.reshape(bsz, seq, d)


def setup_inputs(seed: int = 0) -> dict:
    key = jax.random.key(seed)
    ks = jax.random.split(key, 16)
    f32 = jnp.float32
    x = jax.random.normal(ks[0], (BATCH, SEQ, D_MODEL), f32)
    norm_mix = 1.0 + 0.01 * jax.random.normal(ks[1], (DEPTH, D_MODEL), f32)
    w_in = jax.random.normal(ks[2], (DEPTH, D_MODEL, IN_PROJ_WIDTH), f32) * D_MODEL ** -0.5
    conv_w = jax.random.normal(ks[3], (DEPTH, CONV_K, CONV_WIDTH), f32) * CONV_K ** -0.5
    conv_b = 0.02 * jax.random.normal(ks[4], (DEPTH, CONV_WIDTH), f32)
    pool_w = jax.random.normal(ks[5], (DEPTH, POOL_GROUPS, POOL_GROUP_DIM, POOL_GROUP_DIM), f32) * POOL_GROUP_DIM ** -0.5
    pool_scale = 1.0 + 0.1 * jax.random.normal(ks[6], (DEPTH, POOL_WIDTH), f32)
    w_out = jax.random.normal(ks[7], (DEPTH, D_MIX, D_MODEL), f32) * D_MIX ** -0.5
    norm_ffn = 1.0 + 0.01 * jax.random.normal(ks[8], (DEPTH, D_MODEL), f32)
    peer_w_q = jax.random.normal(ks[9], (DEPTH, D_MODEL, PEER_HEADS * PEER_D_KEY), f32) * D_MODEL ** -0.5
    peer_sub_keys = jax.random.normal(ks[10], (DEPTH, PEER_HEADS, 2, PEER_N_KEYS, PEER_HALF), f32) * PEER_HALF ** -0.5
    peer_u = jax.random.normal(ks[11], (DEPTH, PEER_N_EXPERTS, D_MODEL), f32) * D_MODEL ** -0.5
    peer_v = jax.random.normal(ks[12], (DEPTH, PEER_N_EXPERTS, D_MODEL), f32) * 0.5
    norm_final = 1.0 + 0.01 * jax.random.normal(ks[13], (D_MODEL,), f32)
    return {"x": x, "norm_mix": norm_mix, "w_in": w_in, "conv_w": conv_w, "conv_b": conv_b,
            "pool_w": pool_w, "pool_scale": pool_scale, "w_out": w_out, "norm_ffn": norm_ffn,
            "peer_w_q": peer_w_q, "peer_sub_keys": peer_sub_keys, "peer_u": peer_u,
            "peer_v": peer_v, "norm_final": norm_final}


def reference(x, norm_mix, w_in, conv_w, conv_b, pool_w, pool_scale, w_out, norm_ffn,
              peer_w_q, peer_sub_keys, peer_u, peer_v, norm_final):
    bsz, seq, _ = x.shape
    for l in range(DEPTH):
        h = rmsnorm(x, norm_mix[l])
        proj = h @ w_in[l]
        gate_b = proj[..., :CONV_WIDTH]
        gate_c = proj[..., CONV_WIDTH:2 * CONV_WIDTH]
        val = proj[..., 2 * CONV_WIDTH:3 * CONV_WIDTH]
        pool_in = proj[..., 3 * CONV_WIDTH:]
        y_conv = gate_b * causal_short_conv(gate_c * val, conv_w[l], conv_b[l])
        pooled = causal_multiscale_pool(pool_in)
        y_pool = jnp.einsum('bsgc,gcd->bsgd', pooled, pool_w[l]).reshape(bsz, seq, POOL_WIDTH) * pool_scale[l]
        mix = jnp.concatenate([y_conv, y_pool], axis=-1) @ w_out[l]
        x = x + mix
        h2 = rmsnorm(x, norm_ffn[l])
        x = x + peer_ffn(h2, peer_w_q[l], peer_sub_keys[l], peer_u[l], peer_v[l])
    return rmsnorm(x, norm_final)
```

```python
import numpy as np
import concourse.bass as bass
import concourse.mybir as mybir
from concourse.bass_utils import run_bass_kernel_spmd

F32 = mybir.dt.float32
BF16 = mybir.dt.bfloat16
U32 = mybir.dt.uint32
ALU = mybir.AluOpType
AF = mybir.ActivationFunctionType
AX = mybir.AxisListType


class _Op:
    __slots__ = ("idx", "eng", "fn", "dma", "dma_count", "waits", "target", "eidx", "ms")


class Prog:
    ENGS = ("pe", "act", "dve", "pool", "sp")

    def __init__(self, nc, ctx):
        self.nc = nc
        self.ctx = ctx
        self.ops = []
        self.by_eng = {e: [] for e in self.ENGS}
        self.last_writer = {}
        self.readers = {}
        self.dma_counts = {}
        self.waited = {e: {} for e in self.ENGS}

    def add(self, eng, fn, reads=(), writes=(), dma=None):
        op = _Op()
        op.idx = len(self.ops)
        op.eng = eng
        op.fn = fn
        op.dma = dma
        op.target = False
        op.ms = None
        op.eidx = len(self.by_eng[eng])
        op.waits = []
        if dma is not None:
            self.dma_counts[dma] = self.dma_counts.get(dma, 0) + 16
            op.dma_count = self.dma_counts[dma]
        else:
            op.dma_count = None
        deps = {}
        for r in reads:
            p = self.last_writer.get(r)
            if p is not None:
                deps[p.idx] = (p, True)
        for w in writes:
            p = self.last_writer.get(w)
            if p is not None and p.idx not in deps:
                deps[p.idx] = (p, False)
            for q in self.readers.get(w, ()):
                if q.idx not in deps:
                    deps[q.idx] = (q, False)
        wd = self.waited[eng]
        best_dma = {}
        best_eng = {}
        for pidx in sorted(deps):
            p, raw = deps[pidx]
            if p.dma is not None:
                if best_dma.get(p.dma, 0) < p.dma_count:
                    best_dma[p.dma] = p.dma_count
            else:
                if p.eng == eng and dma is None:
                    if eng == "pe" or not raw:
                        continue
                if p.eng not in best_eng or best_eng[p.eng].eidx < p.eidx:
                    best_eng[p.eng] = p
        for k, cnt in best_dma.items():
            key = ("dma", k)
            if wd.get(key, 0) >= cnt:
                continue
            wd[key] = cnt
            op.waits.append(("dma", k, cnt))
        for e2, p in best_eng.items():
            key = ("eng", e2)
            if wd.get(key, -1) >= p.eidx:
                continue
            wd[key] = p.eidx
            p.target = True
            op.waits.append(("eng", p))
        for w in writes:
            self.last_writer[w] = op
            self.readers[w] = []
        for r in reads:
            if r not in writes:
                self.readers.setdefault(r, []).append(op)
        self.ops.append(op)
        self.by_eng[eng].append(op)
        return op

    def flush(self, barrier=True):
        nc = self.nc
        if not hasattr(self, "esem"):
            self.esem = {}
            self.dsem = {}
            self.ms_base = {e: 0 for e in self.ENGS}
            self.dma_waited_all = {}
        for e in self.ENGS:
            if e not in self.esem:
                self.esem[e] = self.ctx.enter_context(nc.semaphore("s_" + e))
        for k in self.dma_counts:
            if k not in self.dsem:
                self.dsem[k] = self.ctx.enter_context(nc.semaphore("d_" + str(k)))
        for e in self.ENGS:
            n = self.ms_base[e]
            for op in self.by_eng[e]:
                if op.target:
                    n += 1
                    op.ms = n
            self.ms_base[e] = n
        esem, dsem = self.esem, self.dsem
        dma_final = dict(self.dma_counts)

        def run(e):
            def body(eng):
                for op in self.by_eng[e]:
                    for w in op.waits:
                        if w[0] == "dma":
                            eng.wait_ge(dsem[w[1]], w[2])
                        else:
                            eng.wait_ge(esem[w[1].eng], w[1].ms)
                    if op.fn is None:
                        continue
                    ins = op.fn(eng)
                    if op.dma is not None:
                        ins.then_inc(dsem[op.dma], 16)
                    elif op.target:
                        ins.then_inc(esem[e], 1)
                if e == "sp" and barrier:
                    for k, v in dma_final.items():
                        if self.dma_waited_all.get(k, 0) < v:
                            eng.wait_ge(dsem[k], v)
                            self.dma_waited_all[k] = v
            return body

        with nc.Block() as blk:
            blk.tensor(run("pe"))
            blk.scalar(run("act"))
            blk.vector(run("dve"))
            blk.gpsimd(run("pool"))
            blk.sync(run("sp"))
        if barrier:
            nc.all_engine_barrier()
        self.nops = getattr(self, "nops", 0) + len(self.ops)
        self.ops = []
        self.by_eng = {e: [] for e in self.ENGS}
        self.last_writer = {}
        self.readers = {}
        self.waited = {e: {k: v for k, v in self.waited[e].items() if k[0] == "dma"} for e in self.ENGS}


D = 2048
NTOK = 2048
HALO = 16
NCOL = NTOK + HALO
KC = D // 128
EPS = 1e-6
GELU_TANH = True
NEG = -1.0e30
V_G1, V_G2, V_G3, V_CW, V_CB, V_PS, V_N = 0, 16, 32, 48, 72, 80, 88
C_ID, C_I128, C_I16, C_N = 0, 128, 256, 272
TILES = [(0, HALO)] + [(HALO + i * 512, 512) for i in range(4)]


def build_nc(stop_after=None, debug=False, nt_pass=1024, grp=4):
    nc = bass.Bass("TRN2", target_bir_lowering=False)
    dram = lambda name, shape, dt, kind: nc.dram_tensor(name, shape, dt, kind=kind).ap()
    xT = dram("xT", [D, NCOL], F32, "ExternalInput")
    w_in = dram("w_in", [32, 128, 2048], F32, "ExternalInput")
    w_out = dram("w_out", [16, 128, 2048], F32, "ExternalInput")
    w_q = dram("w_q", [16, 128, 2048], F32, "ExternalInput")
    pool_w = dram("pool_w", [128, 2048], F32, "ExternalInput")
    keysT = dram("keysT", [128, 2048], F32, "ExternalInput")
    vecs = dram("vecs", [128, V_N], F32, "ExternalInput")
    icf = dram("icf", [128, 64], F32, "ExternalInput")
    cst = dram("cst", [128, C_N], F32, "ExternalInput")
    if stop_after is None or stop_after.startswith("B"):
        UT = dram("UT", [128, 128, 2048], F32, "ExternalInput")
        VP = dram("VP", [128, 128, 2048], F32, "ExternalInput")
    outT = dram("outT", [D, NTOK], F32, "ExternalOutput")
    skind = "ExternalOutput" if debug else "Internal"
    x1T = dram("x1T", [D, NTOK], F32, skind)
    h2T_d = dram("h2T_d", [KC, 128, NTOK], BF16, skind)
    GT = dram("GT", [128, 128, NTOK], BF16, skind)
    dbg = dram("dbg", [128, 4096], F32, "ExternalOutput") if debug else None

    from contextlib import ExitStack
    with ExitStack() as top:
        P = Prog(nc, top)
        sb = lambda ctx, name, shape, dt: ctx.enter_context(nc.sbuf_tensor(name, shape, dt))
        ps = [top.enter_context(nc.psum_tensor(f"ps{i}", [128, 512], F32)) for i in range(8)]
        vec_sb = sb(top, "vec_sb", [128, V_N], F32)
        icf_sb = sb(top, "icf_sb", [128, 64], F32)
        cst_sb = sb(top, "cst_sb", [128, C_N], F32)
        ones_bf = sb(top, "ones_bf", [128, 128], BF16)
        rstdB = sb(top, "rstdB", [128, NTOK], F32)
        P.add("sp", lambda e: e.dma_start(out=vec_sb[:], in_=vecs), writes=["vec"], dma="c0")
        P.add("sp", lambda e: e.dma_start(out=icf_sb[:], in_=icf), writes=["icf"], dma="c1")
        P.add("sp", lambda e: e.dma_start(out=cst_sb[:], in_=cst), writes=["cst"], dma="c2")
        P.add("pool", lambda e: e.memset(ones_bf[:], 1.0), writes=["ones"])

        def vcol(off, k):
            return vec_sb[:, off + k: off + k + 1]

        with ExitStack() as phA:
            hT = sb(phA, "hT", [128, KC, NCOL], BF16)
            ycat = sb(phA, "ycat", [128, KC, NTOK], BF16)
            rstd = sb(phA, "rstd", [128, NCOL], F32)
            rstd2 = sb(phA, "rstd2", [128, NCOL], F32)
            with ExitStack() as st:
                xin = [sb(st, f"xin{i}", [128, NCOL], F32) for i in range(2)]
                xsq = [sb(st, f"xsq{i}", [128, NCOL], BF16) for i in range(2)]
                sq = sb(st, "sqtmp", [128, NCOL], F32)
                for k in range(KC):
                    s = k % 2
                    P.add("sp", lambda e, k=k, s=s: e.dma_start(out=xin[s][:], in_=xT[k * 128:(k + 1) * 128, :]),
                          writes=[f"xin{s}"], dma=f"xin{s}")
                    P.add("dve", lambda e, k=k, s=s: e.tensor_scalar(out=hT[:, k, :], in0=xin[s][:], scalar1=vcol(V_G1, k),
                                                                     scalar2=None, op0=ALU.mult),
                          reads=[f"xin{s}", "vec"], writes=[f"hT{k}"])
                    P.add("act", lambda e, s=s: e.activation(out=xsq[s][:], in_=xin[s][:], func=AF.Square),
                          reads=[f"xin{s}"], writes=[f"xsq{s}"])
                    for cb in range(5):
                        c0 = cb * 512
                        n = min(512, NCOL - c0)
                        P.add("pe", lambda e, s=s, cb=cb, c0=c0, n=n, k=k: e.matmul(
                            ps[cb][:, 0:n], lhsT=ones_bf[:], rhs=xsq[s][:, c0:c0 + n], start=(k == 0), stop=(k == KC - 1)),
                              reads=[f"xsq{s}", "ones"], writes=[f"ps{cb}"])
                for cb in range(5):
                    c0 = cb * 512
                    n = min(512, NCOL - c0)
                    P.add("act", lambda e, cb=cb, c0=c0, n=n: e.activation(out=sq[:, c0:c0 + n], in_=ps[cb][:, 0:n], func=AF.Sqrt,
                                                                          scale=1.0 / D, bias=EPS),
                          reads=[f"ps{cb}"], writes=[f"sq{cb}"])
                    P.add("dve", lambda e, c0=c0, n=n: e.reciprocal(out=rstd[:, c0:c0 + n], in_=sq[:, c0:c0 + n]),
                          reads=[f"sq{cb}"], writes=[f"rstd{cb}"])
                    P.add("pool", lambda e, c0=c0, n=n: e.tensor_tensor(out=rstd2[:, c0:c0 + n], in0=rstd[:, c0:c0 + n],
                                                                        in1=rstd[:, c0:c0 + n], op=ALU.mult),
                          reads=[f"rstd{cb}"], writes=[f"rstd2{cb}"])
                P.flush()
            if stop_after == "A1a":
                return _finish(nc, P, dbg, [(rstd, 0, NCOL)])

            with ExitStack() as st:
                wA = [sb(st, f"wA{i}", [128, 3, KC, 128], BF16) for i in range(2)]
                cs = [sb(st, f"cs{i}", [128, 512], F32) for i in range(2)]
                cvb = [sb(st, f"cv{i}", [128, NCOL], F32) for i in range(2)]
                acc = [sb(st, f"acc{i}", [128, 512], F32) for i in range(2)]
                acc2 = [sb(st, f"accb{i}", [128, 512], F32) for i in range(2)]
                bank = 0
                it = 0
                def loadA(j):
                    s = j % 2
                    for w3, chunk in enumerate((j, 8 + j, 16 + j)):
                        P.add("pool", lambda e, s=s, w3=w3, chunk=chunk: e.dma_start(
                            out=wA[s][:, w3].rearrange("p k n -> p (k n)"), in_=w_in[chunk], max_dma_last_dim=4096),
                              writes=[f"wA{s}_{w3}"], dma=f"wA{s}_{w3}")
                loadA(0)
                for j in range(8):
                    s = j % 2
                    if j + 1 < 8:
                        loadA(j + 1)
                    for ti, (c0, n) in enumerate(TILES):
                        halo = (ti == 0)
                        bC, bV, bB = bank % 8, (bank + 1) % 8, (bank + 2) % 8
                        bank += 2 if halo else 3
                        for w3, bk in ((1, bC), (2, bV)) + (() if halo else ((0, bB),)):
                            for k in range(KC):
                                P.add("pe", lambda e, s=s, w3=w3, bk=bk, k=k, c0=c0, n=n: e.matmul(
                                    ps[bk][:, 0:n], lhsT=wA[s][:, w3, k, :], rhs=hT[:, k, c0:c0 + n], start=(k == 0), stop=(k == KC - 1)),
                                      reads=[f"wA{s}_{w3}", f"hT{k}"], writes=[f"ps{bk}"])
                        u = it % 2
                        it += 1
                        cvres = f"cv{s}_{ti}"
                        P.add("dve", lambda e, u=u, bC=bC, c0=c0, n=n: e.tensor_tensor(out=cs[u][:, 0:n], in0=ps[bC][:, 0:n],
                                                                                    in1=rstd2[:, c0:c0 + n], op=ALU.mult),
                              reads=[f"ps{bC}"], writes=[f"cs{u}"])
                        P.add("dve", lambda e, u=u, bV=bV, c0=c0, n=n, s=s: e.tensor_tensor(out=cvb[s][:, c0:c0 + n], in0=ps[bV][:, 0:n],
                                                                                         in1=cs[u][:, 0:n], op=ALU.mult),
                              reads=[f"ps{bV}", f"cs{u}"], writes=[cvres])
                        if halo:
                            continue
                        prev = f"cv{s}_{ti - 1}"
                        cw = lambda kk, j=j: vec_sb[:, V_CW + j * 3 + kk: V_CW + j * 3 + kk + 1]
                        P.add("pool", lambda e, u=u, s=s, c0=c0, n=n, j=j, cw=cw: e.tensor_scalar(
                            out=acc[u][:, 0:n], in0=cvb[s][:, c0:c0 + n], scalar1=cw(2), scalar2=vcol(V_CB, j), op0=ALU.mult, op1=ALU.add),
                              reads=[cvres, "vec"], writes=[f"acc{u}"])
                        P.add("dve", lambda e, u=u, s=s, c0=c0, n=n, cw=cw: e.scalar_tensor_tensor(
                            out=acc[u][:, 0:n], in0=cvb[s][:, c0 - 1:c0 - 1 + n], scalar=cw(1), in1=acc[u][:, 0:n], op0=ALU.mult, op1=ALU.add),
                              reads=[cvres, prev, f"acc{u}"], writes=[f"acc{u}"])
                        P.add("dve", lambda e, u=u, s=s, c0=c0, n=n, cw=cw: e.scalar_tensor_tensor(
                            out=acc[u][:, 0:n], in0=cvb[s][:, c0 - 2:c0 - 2 + n], scalar=cw(0), in1=acc[u][:, 0:n], op0=ALU.mult, op1=ALU.add),
                              reads=[cvres, prev, f"acc{u}"], writes=[f"acc{u}"])
                        P.add("pool", lambda e, u=u, c0=c0, n=n: e.tensor_tensor(out=acc2[u][:, 0:n], in0=acc[u][:, 0:n],
                                                                                in1=rstd[:, c0:c0 + n], op=ALU.mult),
                              reads=[f"acc{u}"], writes=[f"accb{u}"])
                        P.add("dve", lambda e, u=u, bB=bB, c0=c0, n=n, j=j: e.tensor_tensor(
                            out=ycat[:, j, c0 - HALO:c0 - HALO + n], in0=ps[bB][:, 0:n], in1=acc2[u][:, 0:n], op=ALU.mult),
                              reads=[f"ps{bB}", f"accb{u}"], writes=[f"ycat{j}_{ti}"])
                P.flush()
            if stop_after == "A1b":
                return _finish(nc, P, dbg, [(ycat[:, 0, :], 0, NTOK), (ycat[:, 7, :], 0, NTOK)])

            with ExitStack() as st:
                wP = [sb(st, f"wP{i}", [128, KC, 128], BF16) for i in range(2)]
                pw = sb(st, "pw", [128, 4, 2, 256], BF16)
                zb = [sb(st, f"z{i}", [128, NCOL], F32) for i in range(2)]
                sA = [sb(st, f"sA{i}", [128, NCOL], F32) for i in range(1)]
                sB = [sb(st, f"sB{i}", [128, NCOL], F32) for i in range(1)]
                pooled = [sb(st, f"pooled{i}", [128, NTOK], BF16) for i in range(2)]
                tmp16 = [sb(st, f"tmp16_{i}", [128, 16], F32) for i in range(2)]
                P.add("pool", lambda e: e.dma_start(out=pw[:].rearrange("p g c d -> p (g c d)"), in_=pool_w, max_dma_last_dim=4096),
                      writes=["pw"], dma="pw")
                bank = 0

                def loadP(j):
                    s = j % 2
                    P.add("pool", lambda e, s=s, j=j: e.dma_start(out=wP[s][:].rearrange("p k n -> p (k n)"), in_=w_in[24 + j],
                                                                  max_dma_last_dim=4096),
                          writes=[f"wP{s}"], dma=f"wP{s}")
                for g, win in enumerate((2, 4, 8, 16)):
                    for jj in range(2):
                        j = 2 * g + jj
                        s = j % 2
                        zi = jj
                        if j == 0:
                            loadP(0)
                        if j + 1 < 8:
                            loadP(j + 1)
                        for ti, (c0, n) in enumerate(TILES):
                            bk = bank % 8
                            bank += 1
                            for k in range(KC):
                                P.add("pe", lambda e, s=s, bk=bk, k=k, c0=c0, n=n: e.matmul(
                                    ps[bk][:, 0:n], lhsT=wP[s][:, k, :], rhs=hT[:, k, c0:c0 + n], start=(k == 0), stop=(k == KC - 1)),
                                      reads=[f"wP{s}", f"hT{k}"], writes=[f"ps{bk}"])
                            P.add("dve", lambda e, zi=zi, bk=bk, c0=c0, n=n: e.tensor_tensor(out=zb[zi][:, c0:c0 + n], in0=ps[bk][:, 0:n],
                                                                                          in1=rstd[:, c0:c0 + n], op=ALU.mult),
                                  reads=[f"ps{bk}"], writes=[f"z{zi}"])
                        z = zb[zi]
                        cur, cur_name, sh = z, f"z{zi}", 1
                        bufs = [(sA[0], "sA0"), (sB[0], "sB0")]
                        bi = 0
                        while sh < win:
                            dst, dname = bufs[bi % 2]
                            bi += 1
                            lo = 2 * sh - 1
                            P.add("dve", lambda e, dst=dst, cur=cur, lo=lo, sh=sh: e.tensor_tensor(
                                out=dst[:, lo:NCOL], in0=cur[:, lo:NCOL], in1=cur[:, lo - sh:NCOL - sh], op=ALU.add),
                                  reads=[cur_name], writes=[dname])
                            cur, cur_name = dst, dname
                            sh *= 2
                        pi = jj
                        P.add("dve", lambda e, pi=pi, cur=cur, z=z, win=win: e.scalar_tensor_tensor(
                            out=pooled[pi][:, 16:NTOK], in0=cur[:, HALO + 16:NCOL], scalar=1.0 / win, in1=z[:, HALO + 16:NCOL],
                            op0=ALU.mult, op1=ALU.subtract),
                              reads=[cur_name, f"z{zi}"], writes=[f"pooled{pi}"])
                        P.add("dve", lambda e, jj=jj, cur=cur, g=g: e.tensor_tensor(
                            out=tmp16[jj][:], in0=cur[:, HALO:HALO + 16], in1=icf_sb[:, g * 16:(g + 1) * 16], op=ALU.mult),
                              reads=[cur_name, "icf"], writes=[f"tmp16_{jj}"])
                        P.add("dve", lambda e, jj=jj, pi=pi, z=z: e.tensor_tensor(
                            out=pooled[pi][:, 0:16], in0=tmp16[jj][:], in1=z[:, HALO:HALO + 16], op=ALU.subtract),
                              reads=[f"tmp16_{jj}", f"z{zi}"], writes=[f"pooledh{pi}"])
                    for ti in range(4):
                        c0 = ti * 512
                        for dout in range(2):
                            bk = bank % 8
                            bank += 1
                            for cc in range(2):
                                pi = cc
                                P.add("pe", lambda e, g=g, cc=cc, dout=dout, bk=bk, pi=pi, c0=c0: e.matmul(
                                    ps[bk][:, 0:512], lhsT=pw[:, g, cc, dout * 128:(dout + 1) * 128], rhs=pooled[pi][:, c0:c0 + 512],
                                    start=(cc == 0), stop=(cc == 1)),
                                      reads=["pw", f"pooled{pi}", f"pooledh{pi}"], writes=[f"ps{bk}"])
                            jo = 8 + 2 * g + dout
                            P.add("dve", lambda e, bk=bk, jo=jo, c0=c0, g=g, dout=dout: e.tensor_scalar(
                                out=ycat[:, jo, c0:c0 + 512], in0=ps[bk][:, 0:512], scalar1=vcol(V_PS, 2 * g + dout), scalar2=None, op0=ALU.mult),
                                  reads=[f"ps{bk}", "vec"], writes=[f"ycat{jo}_{ti}"])
                P.flush()
            if stop_after == "A1c":
                return _finish(nc, P, dbg, [(ycat[:, 8, :], 0, NTOK), (ycat[:, 15, :], 0, NTOK)])

            with ExitStack() as st:
                wO = [sb(st, f"wO{i}", [128, KC, 128], BF16) for i in range(2)]
                xres = [sb(st, f"xres{i}", [128, NTOK], F32) for i in range(2)]
                x1sq = [sb(st, f"x1sq{i}", [128, 512], BF16) for i in range(4)]
                pending = [None]
                sq2 = sb(st, "sq2", [128, NTOK], F32)
                bank = 0
                it = 0
                def loadO(m):
                    s = m % 2
                    P.add("pool", lambda e, s=s, m=m: e.dma_start(out=wO[s][:].rearrange("p k n -> p (k n)"), in_=w_out[m], max_dma_last_dim=4096),
                          writes=[f"wO{s}"], dma=f"wO{s}")
                loadO(0)
                for m in range(KC):
                    s = m % 2
                    if m + 1 < KC:
                        loadO(m + 1)
                    xr4 = [f"xres{s}_{ti}" for ti in range(4)]
                    P.add("sp", lambda e, s=s, m=m: e.dma_start(out=xres[s][:], in_=xT[m * 128:(m + 1) * 128, HALO:NCOL]),
                          writes=xr4, dma=f"xres{s}")
                    for ti in range(4):
                        c0 = ti * 512
                        bk = bank % 4
                        bank += 1
                        for k in range(KC):
                            P.add("pe", lambda e, s=s, bk=bk, k=k, c0=c0: e.matmul(
                                ps[bk][:, 0:512], lhsT=wO[s][:, k, :], rhs=ycat[:, k, c0:c0 + 512], start=(k == 0), stop=(k == KC - 1)),
                                  reads=[f"wO{s}"], writes=[f"ps{bk}"])
                        if pending[0] is not None:
                            pending[0]()
                            pending[0] = None
                        u = it % 4
                        it += 1
                        P.add("dve", lambda e, s=s, bk=bk, c0=c0: e.tensor_tensor(out=xres[s][:, c0:c0 + 512], in0=ps[bk][:, 0:512],
                                                                                 in1=xres[s][:, c0:c0 + 512], op=ALU.add),
                              reads=[f"ps{bk}", xr4[ti]], writes=[xr4[ti]])
                        P.add("act", lambda e, s=s, u=u, c0=c0: e.activation(out=x1sq[u][:], in_=xres[s][:, c0:c0 + 512], func=AF.Square),
                              reads=[xr4[ti]], writes=[f"x1sq{u}"])

                        def _ones(u=u, ti=ti, m=m):
                            P.add("pe", lambda e: e.matmul(ps[4 + ti][:, 0:512], lhsT=ones_bf[:], rhs=x1sq[u][:],
                                                           start=(m == 0), stop=(m == KC - 1)),
                                  reads=[f"x1sq{u}", "ones"], writes=[f"ps{4 + ti}"])
                        pending[0] = _ones
                    P.add("sp", lambda e, s=s, m=m: e.dma_start(out=x1T[m * 128:(m + 1) * 128, :], in_=xres[s][:]),
                          reads=xr4, writes=["x1T"], dma=f"x1st{s}")
                if pending[0] is not None:
                    pending[0]()
                    pending[0] = None
                for ti in range(4):
                    c0 = ti * 512
                    P.add("act", lambda e, ti=ti, c0=c0: e.activation(out=sq2[:, c0:c0 + 512], in_=ps[4 + ti][:, 0:512], func=AF.Sqrt,
                                                                      scale=1.0 / D, bias=EPS),
                          reads=[f"ps{4 + ti}"], writes=[f"sq2_{ti}"])
                    P.add("dve", lambda e, c0=c0: e.reciprocal(out=rstdB[:, c0:c0 + 512], in_=sq2[:, c0:c0 + 512]),
                          reads=[f"sq2_{ti}"], writes=[f"rstdB{ti}"])
                P.flush()
        if stop_after == "A2":
            return _finish(nc, P, dbg, [(rstdB, 0, NTOK)])

        with ExitStack() as phG:
            sv = sb(phG, "sv", [128, 16, 16, 16], F32)
            si = sb(phG, "si", [128, 16, 16, 16], U32)
            with ExitStack() as st:
                h2T = sb(st, "h2T", [128, KC, NTOK], BF16)
                xr = [sb(st, f"xr{i}", [128, NTOK], F32) for i in range(2)]
                wQ = [sb(st, f"wQ{i}", [128, KC, 128], BF16) for i in range(2)]
                qT = [sb(st, f"qT{i}", [128, NTOK], BF16) for i in range(2)]
                keys_bf = sb(st, "keys_bf", [128, 2048], BF16)
                work = [sb(st, f"work{i}", [128, 128], F32) for i in range(16)]
                P.add("pool", lambda e: e.dma_start(out=keys_bf[:], in_=keysT, max_dma_last_dim=4096), writes=["keys"], dma="keys")
                for m in range(KC):
                    s = m % 2
                    P.add("sp", lambda e, s=s, m=m: e.dma_start(out=xr[s][:], in_=x1T[m * 128:(m + 1) * 128, :]),
                          writes=[f"xr{s}"], dma=f"xr{s}")
                    P.add("dve", lambda e, s=s, m=m: e.scalar_tensor_tensor(out=h2T[:, m, :], in0=xr[s][:], scalar=vcol(V_G2, m),
                                                                          in1=rstdB[:], op0=ALU.mult, op1=ALU.mult),
                          reads=[f"xr{s}", "vec"], writes=[f"h2T{m}"])
                    P.add("sp", lambda e, m=m: e.dma_start(out=h2T_d[m], in_=h2T[:, m, :]), reads=[f"h2T{m}"], writes=["h2T_d"],
                          dma=f"h2st{s}")
                bank = 0
                wi = 0
                def loadQ(hp):
                    s = hp % 2
                    P.add("pool", lambda e, s=s, hp=hp: e.dma_start(out=wQ[s][:].rearrange("p k n -> p (k n)"), in_=w_q[hp], max_dma_last_dim=4096),
                          writes=[f"wQ{s}"], dma=f"wQ{s}")
                loadQ(0)
                for hp in range(16):
                    s = hp % 2
                    if hp + 1 < 16:
                        loadQ(hp + 1)
                    if hp == 0:
                        for k in range(KC):
                            for ti in range(4):
                                P.add("pe", lambda e, s=s, k=k, ti=ti: e.matmul(
                                    ps[ti][:, 0:512], lhsT=wQ[s][:, k, :], rhs=h2T[:, k, ti * 512:(ti + 1) * 512], start=(k == 0), stop=(k == KC - 1)),
                                      reads=[f"wQ{s}", f"h2T{k}"], writes=[f"ps{ti}"])
                        for ti in range(4):
                            P.add("act", lambda e, s=s, ti=ti: e.activation(out=qT[s][:, ti * 512:(ti + 1) * 512], in_=ps[ti][:, 0:512], func=AF.Copy),
                                  reads=[f"ps{ti}"], writes=[f"qT{s}_{ti}"])
                    for ti in (range(4) if hp > 0 else ()):
                        c0 = ti * 512
                        bk = bank % 4
                        bank += 1
                        for k in range(KC):
                            P.add("pe", lambda e, s=s, bk=bk, k=k, c0=c0: e.matmul(
                                ps[bk][:, 0:512], lhsT=wQ[s][:, k, :], rhs=h2T[:, k, c0:c0 + 512], start=(k == 0), stop=(k == KC - 1)),
                                  reads=[f"wQ{s}", f"h2T{k}"], writes=[f"ps{bk}"])
                        P.add("act", lambda e, s=s, bk=bk, c0=c0: e.activation(out=qT[s][:, c0:c0 + 512], in_=ps[bk][:, 0:512], func=AF.Copy),
                              reads=[f"ps{bk}"], writes=[f"qT{s}_{ti}"])
                    for stl in range(16):
                        bk = 4 + stl // 4
                        cc = (stl % 4) * 128
                        P.add("pe", lambda e, s=s, stl=stl, bk=bk, cc=cc, hp=hp: e.matmul(
                            ps[bk][:, cc:cc + 128], lhsT=qT[s][:, stl * 128:(stl + 1) * 128], rhs=keys_bf[:, hp * 128:(hp + 1) * 128],
                            start=True, stop=True),
                              reads=[f"qT{s}_{stl // 4}", "keys"], writes=[f"ps{bk}"])
                    scrs = [ps[4 + stl // 4][:, (stl % 4) * 128:(stl % 4) * 128 + 128] for stl in range(16)]
                    bks = [4 + stl // 4 for stl in range(16)]
                    for half in (range(0, 8), range(8, 16)):
                        for stl in half:
                            P.add("dve", lambda e, scr=scrs[stl], stl=stl, hp=hp: e.max(out=sv[:, stl, hp, 0:8], in_=scr),
                                  reads=[f"ps{bks[stl]}"], writes=[f"svA{stl}"])
                        for stl in half:
                            P.add("dve", lambda e, scr=scrs[stl], stl=stl, hp=hp: e.match_replace(out=work[stl][:], in_to_replace=sv[:, stl, hp, 0:8],
                                                                                             in_values=scr, imm_value=NEG),
                                  reads=[f"ps{bks[stl]}", f"svA{stl}"], writes=[f"work{stl}"])
                        for stl in half:
                            P.add("dve", lambda e, scr=scrs[stl], stl=stl, hp=hp: e.max_index(out=si[:, stl, hp, 0:8], in_max=sv[:, stl, hp, 0:8], in_values=scr),
                                  reads=[f"ps{bks[stl]}", f"svA{stl}"], writes=[f"siA{stl}"])
                        for stl in half:
                            P.add("dve", lambda e, stl=stl, hp=hp: e.max(out=sv[:, stl, hp, 8:16], in_=work[stl][:]),
                                  reads=[f"work{stl}"], writes=[f"svB{stl}"])
                        for stl in half:
                            P.add("dve", lambda e, stl=stl, hp=hp: e.max_index(out=si[:, stl, hp, 8:16], in_max=sv[:, stl, hp, 8:16], in_values=work[stl][:]),
                                  reads=[f"work{stl}", f"svB{stl}"], writes=[f"siB{stl}"])
                P.flush()
            if stop_after == "A3a":
                return _finish(nc, P, dbg, [(sv[:, 0].rearrange("p a b -> p (a b)"), 0, 256),
                                            (si[:, 0].rearrange("p a b -> p (a b)"), 0, 256),
                                            (sv[:, 15].rearrange("p a b -> p (a b)"), 0, 256),
                                            (si[:, 15].rearrange("p a b -> p (a b)"), 0, 256)])
            with ExitStack() as st:
                cand = sb(st, "cand", [128, 8, 16, 16], F32)
                work2 = [sb(st, f"work2_{i}", [128, 256], F32) for i in range(4)]
                top = [sb(st, f"top{i}", [128, 8, 16], F32) for i in range(2)]
                pos = [sb(st, f"pos{i}", [128, 8, 16], U32) for i in range(2)]
                dsub = sb(st, "dsub", [128, 8, 16], F32)
                ex = sb(st, "ex", [128, 8, 16], F32)
                zsum = sb(st, "zsum", [128, 8], F32)
                rz = sb(st, "rz", [128, 8], F32)
                gt = sb(st, "gt", [128, 8, 16], F32)
                a_u = sb(st, "a_u", [128, 8, 16], U32)
                b_u = sb(st, "b_u", [128, 8, 16], U32)
                a_f = sb(st, "a_f", [128, 8, 16], F32)
                b_f = sb(st, "b_f", [128, 8, 16], F32)
                si_f = sb(st, "si_f", [128, 8, 2, 16], F32)
                eq = sb(st, "eq", [128, 8, 16, 16], F32)
                sel = sb(st, "sel", [128, 8, 16, 16], F32)
                I_f = sb(st, "I_f", [128, 8, 16], F32)
                J_f = sb(st, "J_f", [128, 8, 16], F32)
                IT = [sb(st, f"IT{i}", [128, 128], BF16) for i in range(2)]
                JT = [sb(st, f"JT{i}", [128, 128], BF16) for i in range(2)]
                gT = [sb(st, f"gT{i}", [128, 128], BF16) for i in range(2)]
                iota_bf = sb(st, "iota_bf", [128, 128], BF16)
                Pm = [sb(st, f"Pm{i}", [128, 128, 32], BF16) for i in range(3)]
                Qm = [sb(st, f"Qm{i}", [128, 128, 32], BF16) for i in range(3)]
                iota3 = sb(st, "iota3", [128, 128, 32], BF16)
                Gsb = [sb(st, f"Gsb{i}", [128, 128, 128], BF16) for i in range(2)]
                ident = cst_sb[:, C_ID:C_ID + 128]
                iota16 = cst_sb[:, C_I16:C_I16 + 16]
                P.add("dve", lambda e: e.tensor_copy(out=iota_bf[:], in_=cst_sb[:, C_I128:C_I128 + 128]), reads=["cst"], writes=["iota_bf"])
                P.add("dve", lambda e: e.tensor_copy(out=iota3[:], in_=iota_bf[:].unsqueeze(2).broadcast_to([128, 128, 32])),
                      reads=["iota_bf"], writes=["iota3"])
                cntG = {'b': 0, 'h': 0}
                def front(stl):
                    u2 = stl % 2
                    svs = sv[:, stl].rearrange("p (h two) k -> p h two k", two=2)
                    sis = si[:, stl].rearrange("p (h two) k -> p h two k", two=2)
                    P.add("pool", lambda e, svs=svs: e.tensor_tensor(
                        out=cand[:], in0=svs[:, :, 0, :].unsqueeze(3).broadcast_to([128, 8, 16, 16]),
                        in1=svs[:, :, 1, :].unsqueeze(2).broadcast_to([128, 8, 16, 16]), op=ALU.add),
                          reads=[], writes=["cand"])
                    chs = [cand[:, h].rearrange("p a b -> p (a b)") for h in range(8)]
                    for hh in (range(0, 4), range(4, 8)):
                        for h in hh:
                            P.add("dve", lambda e, ch=chs[h], h=h, u2=u2: e.max(out=top[u2][:, h, 0:8], in_=ch), reads=["cand"], writes=[f"tA{h}"])
                        for h in hh:
                            P.add("dve", lambda e, ch=chs[h], h=h, u2=u2: e.match_replace(out=work2[h % 4][:], in_to_replace=top[u2][:, h, 0:8], in_values=ch,
                                                                                     imm_value=NEG), reads=["cand", f"tA{h}"], writes=[f"w2_{h % 4}"])
                        for h in hh:
                            P.add("dve", lambda e, ch=chs[h], h=h, u2=u2: e.max_index(out=pos[u2][:, h, 0:8], in_max=top[u2][:, h, 0:8], in_values=ch),
                                  reads=["cand", f"tA{h}"], writes=[f"pA{h}"])
                        for h in hh:
                            P.add("dve", lambda e, h=h, u2=u2: e.max(out=top[u2][:, h, 8:16], in_=work2[h % 4][:]), reads=[f"w2_{h % 4}"], writes=[f"tB{h}"])
                        for h in hh:
                            P.add("dve", lambda e, h=h, u2=u2: e.max_index(out=pos[u2][:, h, 8:16], in_max=top[u2][:, h, 8:16], in_values=work2[h % 4][:]),
                                  reads=[f"w2_{h % 4}", f"tB{h}"], writes=[f"pB{h}"])
                        yield
                    tall = [f"tA{h}" for h in range(8)] + [f"tB{h}" for h in range(8)]
                    pall = [f"pA{h}" for h in range(8)] + [f"pB{h}" for h in range(8)]
                    P.add("pool", lambda e, u2=u2: e.tensor_tensor(out=dsub[:], in0=top[u2][:], in1=top[u2][:, :, 0:1].broadcast_to([128, 8, 16]),
                                                                  op=ALU.subtract), reads=tall, writes=["dsub"])
                    P.add("act", lambda e: e.activation(out=ex[:], in_=dsub[:], func=AF.Exp), reads=["dsub"], writes=["ex"])
                    P.add("dve", lambda e: e.tensor_reduce(out=zsum[:], in_=ex[:], axis=AX.X, op=ALU.add), reads=["ex"], writes=["zsum"])
                    P.add("dve", lambda e: e.reciprocal(out=rz[:], in_=zsum[:]), reads=["zsum"], writes=["rz"])
                    P.add("pool", lambda e: e.tensor_tensor(out=gt[:], in0=ex[:], in1=rz[:].unsqueeze(2).broadcast_to([128, 8, 16]), op=ALU.mult),
                          reads=["ex", "rz"], writes=["gt"])
                    P.add("dve", lambda e, u2=u2: e.tensor_single_scalar(out=a_u[:], in_=pos[u2][:], scalar=4, op=ALU.logical_shift_right),
                          reads=pall, writes=["a_u"])
                    P.add("dve", lambda e, u2=u2: e.tensor_single_scalar(out=b_u[:], in_=pos[u2][:], scalar=15, op=ALU.bitwise_and),
                          reads=pall, writes=["b_u"])
                    P.add("dve", lambda e: e.tensor_copy(out=a_f[:], in_=a_u[:]), reads=["a_u"], writes=["a_f"])
                    P.add("dve", lambda e: e.tensor_copy(out=b_f[:], in_=b_u[:]), reads=["b_u"], writes=["b_f"])
                    P.add("dve", lambda e, sis=sis: e.tensor_copy(out=si_f[:], in_=sis), reads=[], writes=["si_f"])
                    dec = ((a_f, "a_f", I_f, "I_f", eq, "eq"), (b_f, "b_f", J_f, "J_f", cand, "cand"))
                    for which, (xf, xname, dst, dname, ebuf, ename) in enumerate(dec):
                        P.add("dve", lambda e, xf=xf, ebuf=ebuf: e.tensor_tensor(
                            out=ebuf[:], in0=xf[:].unsqueeze(3).broadcast_to([128, 8, 16, 16]),
                            in1=iota16.unsqueeze(1).unsqueeze(1).broadcast_to([128, 8, 16, 16]), op=ALU.is_equal),
                              reads=[xname, "cst"], writes=[ename])
                    for which, (xf, xname, dst, dname, ebuf, ename) in enumerate(dec):
                        P.add("pool", lambda e, which=which, ebuf=ebuf: e.tensor_tensor(
                            out=sel[:], in0=ebuf[:], in1=si_f[:, :, which, :].unsqueeze(2).broadcast_to([128, 8, 16, 16]), op=ALU.mult),
                              reads=[ename, "si_f"], writes=["sel"])
                        P.add("dve", lambda e, dst=dst: e.tensor_reduce(out=dst[:], in_=sel[:], axis=AX.X, op=ALU.add), reads=["sel"], writes=[dname])
                    yield
                    tb = stl % 2
                    for ci, (src, sname) in enumerate(((I_f, "I_f"), (J_f, "J_f"), (gt, "gt"))):
                        P.add("pe", lambda e, tb=tb, ci=ci, src=src: e.transpose(out=ps[tb][:, ci * 128:(ci + 1) * 128],
                                                                               in_=src[:].rearrange("p h k -> p (h k)"), identity=ident),
                              reads=[sname, "cst"], writes=[f"ps{tb}"])
                    for ci, (dst, dname) in enumerate(((IT[u2], f"IT{u2}"), (JT[u2], f"JT{u2}"), (gT[u2], f"gT{u2}"))):
                        P.add("act", lambda e, tb=tb, ci=ci, dst=dst: e.activation(out=dst[:], in_=ps[tb][:, ci * 128:(ci + 1) * 128], func=AF.Copy),
                              reads=[f"ps{tb}"], writes=[dname])
                def back(stl, gen):
                    u2 = stl % 2
                    g2 = stl % 2
                    for qtr in range(4):
                        r = cntG['h'] % 3
                        cntG['h'] += 1
                        t0 = qtr * 32
                        P.add("dve", lambda e, r=r, u2=u2, t0=t0: e.tensor_tensor(
                            out=Qm[r][:], in0=iota3[:], in1=JT[u2][:, t0:t0 + 32].unsqueeze(1).broadcast_to([128, 128, 32]), op=ALU.is_equal),
                              reads=[f"JT{u2}", "iota3"], writes=[f"Qm{r}"])
                        P.add("dve", lambda e, r=r, u2=u2, t0=t0: e.tensor_tensor(
                            out=Pm[r][:], in0=iota3[:], in1=IT[u2][:, t0:t0 + 32].unsqueeze(1).broadcast_to([128, 128, 32]), op=ALU.is_equal),
                              reads=[f"IT{u2}", "iota3"], writes=[f"Pm{r}"])
                        P.add("dve", lambda e, r=r, u2=u2, t0=t0: e.tensor_tensor(
                            out=Pm[r][:], in0=Pm[r][:], in1=gT[u2][:, t0:t0 + 32].unsqueeze(1).broadcast_to([128, 128, 32]), op=ALU.mult),
                              reads=[f"Pm{r}", f"gT{u2}"], writes=[f"Pm{r}"])
                        if gen is not None:
                            next(gen, None)
                        for q4 in range(8):
                            bk = 2 + cntG['b'] % 6
                            cntG['b'] += 1
                            for tt in range(4):
                                tl = q4 * 4 + tt
                                P.add("pe", lambda e, r=r, bk=bk, tt=tt, tl=tl: e.matmul(
                                    ps[bk][:, tt * 128:(tt + 1) * 128], lhsT=Pm[r][:, :, tl], rhs=Qm[r][:, :, tl], start=True, stop=True),
                                      reads=[f"Pm{r}", f"Qm{r}"], writes=[f"ps{bk}"])
                            tg = t0 + q4 * 4
                            dstv = Gsb[g2][:, :, tg:tg + 4]
                            srcv = ps[bk][:, 0:512].rearrange("p (t j) -> p j t", t=4)
                            P.add("act", lambda e, dstv=dstv, srcv=srcv: e.activation(out=dstv, in_=srcv, func=AF.Copy),
                                  reads=[f"ps{bk}"], writes=[f"Gsb{g2}_{qtr}_{q4}"])
                    allg = [f"Gsb{g2}_{qtr}_{q4}" for qtr in range(4) for q4 in range(8)]
                    for jb in range(8):
                        j0 = jb * 16
                        P.add("sp", lambda e, g2=g2, j0=j0, stl=stl: e.dma_start(
                            out=GT[j0:j0 + 16, :, stl * 128:(stl + 1) * 128].rearrange("j i t -> i j t"), in_=Gsb[g2][:, j0:j0 + 16, :]),
                              reads=allg, writes=["GT"], dma=f"gst{g2}_{jb % 2}")
                for _ in front(0):
                    pass
                for stl in range(16):
                    gen = front(stl + 1) if stl + 1 < 16 else None
                    back(stl, gen)
                    if gen is not None:
                        for _ in gen:
                            pass
                P.flush()
            if stop_after == "A3b":
                return _finish(nc, P, dbg, [(I_f[:].rearrange("p h k -> p (h k)"), 0, 128), (J_f[:].rearrange("p h k -> p (h k)"), 0, 128),
                                            (gt[:].rearrange("p h k -> p (h k)"), 0, 128)])
        GELU = AF.Gelu_apprx_tanh if GELU_TANH else AF.Gelu
        NTB = nt_pass // 512
        NG = 128 // grp
        with ExitStack() as phB:
            h2p = sb(phB, "h2p", [128, KC, nt_pass], BF16)
            accb = sb(phB, "accB", [128, KC, nt_pass], F32)
            UTb = [sb(phB, f"UTb{i}", [128, KC, 128], BF16) for i in range(3)]
            Vb = [[sb(phB, f"Vb{g}_{i}", [128, D], BF16) for i in range(grp)] for g in range(2)]
            Gc = [[sb(phB, f"Gc{g}_{i}", [128, nt_pass], BF16) for i in range(grp)] for g in range(2)]
            GA = [[sb(phB, f"GA{g}_{i}", [128, nt_pass], BF16) for i in range(grp)] for g in range(2)]
            gel = [sb(phB, f"gel{i}", [128, 512], BF16) for i in range(2)]
            xf = [sb(phB, f"xf{i}", [128, nt_pass], F32) for i in range(2)]
            ysq = [sb(phB, f"ysq{i}", [128, 512], BF16) for i in range(2)]
            sq3 = sb(phB, "sq3", [128, nt_pass], F32)
            r3 = sb(phB, "r3", [128, nt_pass], F32)
            cnt = {"a": 0, "b": 0, "g": 0, "u": 0}
            npass = NTOK // nt_pass
            if True:
                def load_h2p(tp0):
                    P.add("sp", lambda e, tp0=tp0: e.dma_start(out=h2p[:], in_=h2T_d[:, :, tp0:tp0 + nt_pass].rearrange("k p t -> p k t")),
                          writes=["h2p"], dma="h2p")

                def mm1(gi, tp0):
                    gs = gi % 2
                    for ci in range(grp):
                        c = gi * grp + ci
                        us = cnt["u"] % 3
                        cnt["u"] += 1
                        P.add("pool", lambda e, us=us, c=c: e.dma_start(out=UTb[us][:].rearrange("p k n -> p (k n)"), in_=UT[c], max_dma_last_dim=4096),
                              writes=[f"UTb{us}"], dma=f"UTb{us}")
                        P.add("pool", lambda e, gs=gs, ci=ci, c=c: e.dma_start(out=Vb[gs][ci][:], in_=VP[c], max_dma_last_dim=4096),
                              writes=[f"Vb{gs}_{ci}"], dma=f"Vb{gs}_{ci}")
                        P.add("sp", lambda e, gs=gs, ci=ci, c=c, tp0=tp0: e.dma_start(out=Gc[gs][ci][:], in_=GT[c, :, tp0:tp0 + nt_pass]),
                              writes=[f"Gc{gs}_{ci}"], dma=f"Gc{gs}_{ci}")
                        for tb in range(NTB):
                            a = cnt["a"] % 2
                            cnt["a"] += 1
                            for k in range(KC):
                                P.add("pe", lambda e, us=us, a=a, k=k, tb=tb: e.matmul(
                                    ps[a][:, 0:512], lhsT=UTb[us][:, k, :], rhs=h2p[:, k, tb * 512:(tb + 1) * 512], start=(k == 0), stop=(k == KC - 1)),
                                      reads=[f"UTb{us}", "h2p"], writes=[f"ps{a}"])
                            x = cnt["g"] % 2
                            cnt["g"] += 1
                            P.add("act", lambda e, a=a, x=x: e.activation(out=gel[x][:], in_=ps[a][:, 0:512], func=GELU),
                                  reads=[f"ps{a}"], writes=[f"gel{x}"])
                            P.add("dve", lambda e, gs=gs, ci=ci, x=x, tb=tb: e.tensor_tensor(
                                out=GA[gs][ci][:, tb * 512:(tb + 1) * 512], in0=gel[x][:], in1=Gc[gs][ci][:, tb * 512:(tb + 1) * 512], op=ALU.mult),
                                  reads=[f"gel{x}", f"Gc{gs}_{ci}"], writes=[f"GA{gs}_{ci}_{tb}"])

                def mm2(gi):
                    gs = gi % 2
                    for m in range(KC):
                        for tb in range(NTB):
                            b = 2 + cnt["b"] % 6
                            cnt["b"] += 1
                            for ci in range(grp):
                                P.add("pe", lambda e, gs=gs, ci=ci, m=m, tb=tb, b=b: e.matmul(
                                    ps[b][:, 0:512], lhsT=Vb[gs][ci][:, m * 128:(m + 1) * 128], rhs=GA[gs][ci][:, tb * 512:(tb + 1) * 512],
                                    start=(ci == 0), stop=(ci == grp - 1)),
                                      reads=[f"Vb{gs}_{ci}", f"GA{gs}_{ci}_{tb}"], writes=[f"ps{b}"])
                            dst = accb[:, m, tb * 512:(tb + 1) * 512]
                            if gi == 0:
                                P.add("act", lambda e, dst=dst, b=b: e.activation(out=dst, in_=ps[b][:, 0:512], func=AF.Copy),
                                      reads=[f"ps{b}"], writes=[f"acc{m}_{tb}"])
                            else:
                                P.add("dve", lambda e, dst=dst, b=b: e.tensor_tensor(out=dst, in0=ps[b][:, 0:512], in1=dst, op=ALU.add),
                                      reads=[f"ps{b}", f"acc{m}_{tb}"], writes=[f"acc{m}_{tb}"])

                def tail(tp0):
                  for m in range(KC):
                    s = m % 2
                    P.add("sp", lambda e, s=s, m=m, tp0=tp0: e.dma_start(out=xf[s][:], in_=x1T[m * 128:(m + 1) * 128, tp0:tp0 + nt_pass]),
                          writes=[f"xf{s}"], dma=f"xf{s}")
                    accs = [f"acc{m}_{tb}" for tb in range(NTB)]
                    P.add("dve", lambda e, s=s, m=m: e.tensor_tensor(out=accb[:, m, :], in0=accb[:, m, :], in1=xf[s][:], op=ALU.add),
                          reads=[f"xf{s}"] + accs, writes=accs)
                    for tb in range(NTB):
                        y = (m * NTB + tb) % 2
                        P.add("act", lambda e, y=y, m=m, tb=tb: e.activation(out=ysq[y][:], in_=accb[:, m, tb * 512:(tb + 1) * 512], func=AF.Square),
                              reads=[f"acc{m}_{tb}"], writes=[f"ysq{y}"])
                        P.add("pe", lambda e, y=y, m=m, tb=tb: e.matmul(ps[6 + tb][:, 0:512], lhsT=ones_bf[:], rhs=ysq[y][:],
                                                                       start=(m == 0), stop=(m == KC - 1)),
                              reads=[f"ysq{y}", "ones"], writes=[f"ps{6 + tb}"])
                  for tb in range(NTB):
                    P.add("act", lambda e, tb=tb: e.activation(out=sq3[:, tb * 512:(tb + 1) * 512], in_=ps[6 + tb][:, 0:512], func=AF.Sqrt,
                                                               scale=1.0 / D, bias=EPS), reads=[f"ps{6 + tb}"], writes=[f"sq3_{tb}"])
                    P.add("dve", lambda e, tb=tb: e.reciprocal(out=r3[:, tb * 512:(tb + 1) * 512], in_=sq3[:, tb * 512:(tb + 1) * 512]),
                          reads=[f"sq3_{tb}"], writes=[f"r3_{tb}"])
                  for m in range(KC):
                    accs = [f"acc{m}_{tb}" for tb in range(NTB)]
                    P.add("dve", lambda e, m=m: e.scalar_tensor_tensor(out=accb[:, m, :], in0=accb[:, m, :], scalar=vcol(V_G3, m), in1=r3[:],
                                                                      op0=ALU.mult, op1=ALU.mult),
                          reads=accs + [f"r3_{tb}" for tb in range(NTB)] + ["vec"], writes=accs)
                    P.add("sp", lambda e, m=m, tp0=tp0: e.dma_start(out=outT[m * 128:(m + 1) * 128, tp0:tp0 + nt_pass], in_=accb[:, m, :]),
                          reads=accs, writes=["outT"], dma=f"ost{m % 2}")

                load_h2p(0)
                mm1(0, 0)
                for pi in range(npass):
                    tp0 = pi * nt_pass
                    for gi in range(NG):
                        if gi + 1 < NG:
                            mm1(gi + 1, tp0)
                        elif pi + 1 < npass:
                            load_h2p(tp0 + nt_pass)
                            mm1(0, tp0 + nt_pass)
                        mm2(gi)
                    tail(tp0)
                P.flush()
        return _finish(nc, P, dbg, [])


def _finish(nc, P, dbg, taps):
    if dbg is not None:
        off = 0
        for i, (ap, c0, n) in enumerate(taps):
            src = ap[:, c0:c0 + n]
            P.add("pool", lambda e, src=src, off=off, n=n: e.dma_start(out=dbg[:, off:off + n], in_=src, max_dma_last_dim=2048),
                  writes=[f"dbg{i}"], dma=f"dbg{i}")
            off += n
    P.flush()
    return nc


def _host_layout(inputs):
    f = lambda a: np.ascontiguousarray(a, dtype=np.float32)
    x = inputs["x"]
    shared = {}
    wi = inputs["w_in"][0]
    shared["w_in"] = f(wi.reshape(16, 128, 32, 128).transpose(2, 1, 0, 3).reshape(32, 128, 2048))
    wo = inputs["w_out"][0]
    shared["w_out"] = f(wo.reshape(16, 128, 16, 128).transpose(2, 1, 0, 3).reshape(16, 128, 2048))
    wq = inputs["peer_w_q"][0]
    shared["w_q"] = f(wq.reshape(16, 128, 16, 128).transpose(2, 1, 0, 3).reshape(16, 128, 2048))
    pw = inputs["pool_w"][0]
    shared["pool_w"] = f(pw.reshape(4, 2, 128, 256).transpose(2, 0, 1, 3).reshape(128, 2048))
    sk = inputs["peer_sub_keys"][0]
    shared["keysT"] = f(sk.reshape(16, 128, 128).transpose(2, 0, 1).reshape(128, 2048))
    vecs = np.zeros((128, V_N), np.float32)
    vecs[:, V_G1:V_G1 + 16] = inputs["norm_mix"][0].reshape(16, 128).T
    vecs[:, V_G2:V_G2 + 16] = inputs["norm_ffn"][0].reshape(16, 128).T
    vecs[:, V_G3:V_G3 + 16] = inputs["norm_final"].reshape(16, 128).T
    vecs[:, V_CW:V_CW + 24] = inputs["conv_w"][0].reshape(3, 8, 128).transpose(2, 1, 0).reshape(128, 24)
    vecs[:, V_CB:V_CB + 8] = inputs["conv_b"][0].reshape(8, 128).T
    vecs[:, V_PS:V_PS + 8] = inputs["pool_scale"][0].reshape(8, 128).T
    shared["vecs"] = vecs
    cst = np.zeros((128, C_N), np.float32)
    cst[:, C_ID:C_ID + 128] = np.eye(128, dtype=np.float32)
    cst[:, C_I128:C_I128 + 128] = np.arange(128, dtype=np.float32)[None, :]
    cst[:, C_I16:C_I16 + 16] = np.arange(16, dtype=np.float32)[None, :]
    shared["cst"] = cst
    u = inputs["peer_u"][0]
    shared["UT"] = f(u.reshape(128, 128, 16, 128).transpose(1, 3, 2, 0).reshape(128, 128, 2048))
    v = inputs["peer_v"][0]
    shared["VP"] = f(v.reshape(128, 128, 2048).transpose(1, 0, 2))
    in_maps = []
    for c in range(8):
        b, s0 = c // 4, (c % 4) * NTOK
        xs = np.zeros((NCOL, D), np.float32)
        if s0 == 0:
            xs[HALO:] = x[b, :NTOK]
        else:
            xs[:] = x[b, s0 - HALO:s0 + NTOK]
        m = dict(shared)
        m["xT"] = f(xs.T)
        ic = np.zeros((4, 16), np.float32)
        for g, w in enumerate((2, 4, 8, 16)):
            pos = np.arange(s0 + 1, s0 + 17, dtype=np.float32)
            ic[g] = 1.0 / np.minimum(pos, float(w))
        m["icf"] = f(np.broadcast_to(ic.reshape(1, 64), (128, 64)))
        in_maps.append(m)
    return in_maps


def kernel(**inputs):
    in_maps = _host_layout(inputs)
    nc = build_nc()
    res = run_bass_kernel_spmd(nc, in_maps, core_ids=list(range(8)))
    out = np.zeros((2, 8192, D), np.float32)
    for c in range(8):
        b, s0 = c // 4, (c % 4) * NTOK
        out[b, s0:s0 + NTOK] = res.results[c]["outT"].T
    return out
```

```python
import numpy as np
import concourse.bass as bass
import concourse.mybir as mybir
from concourse.bass_utils import run_bass_kernel_spmd

F32 = mybir.dt.float32
BF16 = mybir.dt.bfloat16
U32 = mybir.dt.uint32
ALU = mybir.AluOpType
AF = mybir.ActivationFunctionType
AX = mybir.AxisListType


STRICT_SAME_ENGINE = True


class _Op:
    __slots__ = ("idx", "eng", "fn", "dma", "dma_count", "waits", "target", "eidx", "ms")


class Prog:
    ENGS = ("pe", "act", "dve", "pool", "sp")

    def __init__(self, nc, ctx):
        self.nc = nc
        self.ctx = ctx
        self.ops = []
        self.by_eng = {e: [] for e in self.ENGS}
        self.last_writer = {}
        self.readers = {}
        self.dma_counts = {}
        self.waited = {e: {} for e in self.ENGS}

    def add(self, eng, fn, reads=(), writes=(), dma=None):
        op = _Op()
        op.idx = len(self.ops)
        op.eng = eng
        op.fn = fn
        op.dma = dma
        op.target = False
        op.ms = None
        op.eidx = len(self.by_eng[eng])
        op.waits = []
        if dma is not None:
            self.dma_counts[dma] = self.dma_counts.get(dma, 0) + 16
            op.dma_count = self.dma_counts[dma]
        else:
            op.dma_count = None
        deps = {}
        for r in reads:
            p = self.last_writer.get(r)
            if p is not None:
                deps[p.idx] = (p, True)
        for w in writes:
            p = self.last_writer.get(w)
            if p is not None and p.idx not in deps:
                deps[p.idx] = (p, False)
            for q in self.readers.get(w, ()):
                if q.idx not in deps:
                    deps[q.idx] = (q, False)
        wd = self.waited[eng]
        best_dma = {}
        best_eng = {}
        for pidx in sorted(deps):
            p, raw = deps[pidx]
            if p.dma is not None:
                if best_dma.get(p.dma, 0) < p.dma_count:
                    best_dma[p.dma] = p.dma_count
            else:
                if p.eng == eng and dma is None:
                    if eng == "pe" or not (raw or STRICT_SAME_ENGINE):
                        continue
                if p.eng not in best_eng or best_eng[p.eng].eidx < p.eidx:
                    best_eng[p.eng] = p
        for k, cnt in best_dma.items():
            key = ("dma", k)
            if wd.get(key, 0) >= cnt:
                continue
            wd[key] = cnt
            op.waits.append(("dma", k, cnt))
        for e2, p in best_eng.items():
            key = ("eng", e2)
            if wd.get(key, -1) >= p.eidx:
                continue
            wd[key] = p.eidx
            p.target = True
            op.waits.append(("eng", p))
        for w in writes:
            self.last_writer[w] = op
            self.readers[w] = []
        for r in reads:
            if r not in writes:
                self.readers.setdefault(r, []).append(op)
        self.ops.append(op)
        self.by_eng[eng].append(op)
        return op

    def flush(self, barrier=True):
        nc = self.nc
        if not hasattr(self, "esem"):
            self.esem = {}
            self.dsem = {}
            self.ms_base = {e: 0 for e in self.ENGS}
            self.dma_waited_all = {}
        for e in self.ENGS:
            if e not in self.esem:
                self.esem[e] = self.ctx.enter_context(nc.semaphore("s_" + e))
        for k in self.dma_counts:
            if k not in self.dsem:
                self.dsem[k] = self.ctx.enter_context(nc.semaphore("d_" + str(k)))
        for e in self.ENGS:
            n = self.ms_base[e]
            for op in self.by_eng[e]:
                if op.target:
                    n += 1
                    op.ms = n
            self.ms_base[e] = n
        esem, dsem = self.esem, self.dsem
        dma_final = dict(self.dma_counts)

        def run(e):
            def body(eng):
                for op in self.by_eng[e]:
                    for w in op.waits:
                        if w[0] == "dma":
                            eng.wait_ge(dsem[w[1]], w[2])
                        else:
                            eng.wait_ge(esem[w[1].eng], w[1].ms)
                    if op.fn is None:
                        continue
                    ins = op.fn(eng)
                    if op.dma is not None:
                        ins.then_inc(dsem[op.dma], 16)
                    elif op.target:
                        ins.then_inc(esem[e], 1)
                if e == "sp" and barrier:
                    for k, v in dma_final.items():
                        if self.dma_waited_all.get(k, 0) < v:
                            eng.wait_ge(dsem[k], v)
                            self.dma_waited_all[k] = v
            return body

        with nc.Block() as blk:
            blk.tensor(run("pe"))
            blk.scalar(run("act"))
            blk.vector(run("dve"))
            blk.gpsimd(run("pool"))
            blk.sync(run("sp"))
        if barrier:
            nc.all_engine_barrier()
        self.nops = getattr(self, "nops", 0) + len(self.ops)
        self.ops = []
        self.by_eng = {e: [] for e in self.ENGS}
        self.last_writer = {}
        self.readers = {}
        self.waited = {e: {k: v for k, v in self.waited[e].items() if k[0] == "dma"} for e in self.ENGS}


D = 2048
NTOK = 2048
HALO = 16
NCOL = NTOK + HALO
KC = D // 128
EPS = 1e-6
GELU_TANH = True
NEG = -1.0e30
V_G1, V_G2, V_G3, V_CW, V_CB, V_PS, V_N = 0, 16, 32, 48, 72, 80, 88
C_ID, C_I128, C_I16, C_N = 0, 128, 256, 272
TILES = [(0, HALO)] + [(HALO + i * 512, 512) for i in range(4)]


def build_nc(stop_after=None, debug=False, nt_pass=1024, grp=4):
    nc = bass.Bass("TRN2", target_bir_lowering=False)
    dram = lambda name, shape, dt, kind: nc.dram_tensor(name, shape, dt, kind=kind).ap()
    xT = dram("xT", [D, NCOL], F32, "ExternalInput")
    w_in = dram("w_in", [32, 128, 2048], F32, "ExternalInput")
    w_out = dram("w_out", [16, 128, 2048], F32, "ExternalInput")
    w_q = dram("w_q", [16, 128, 2048], F32, "ExternalInput")
    pool_w = dram("pool_w", [128, 2048], F32, "ExternalInput")
    keysT = dram("keysT", [128, 2048], F32, "ExternalInput")
    vecs = dram("vecs", [128, V_N], F32, "ExternalInput")
    icf = dram("icf", [128, 64], F32, "ExternalInput")
    cst = dram("cst", [128, C_N], F32, "ExternalInput")
    if stop_after is None or stop_after.startswith("B"):
        UT = dram("UT", [128, 128, 2048], F32, "ExternalInput")
        VP = dram("VP", [128, 128, 2048], F32, "ExternalInput")
    outT = dram("outT", [D, NTOK], F32, "ExternalOutput")
    skind = "ExternalOutput" if debug else "Internal"
    x1T = dram("x1T", [D, NTOK], F32, skind)
    h2T_d = dram("h2T_d", [KC, 128, NTOK], BF16, skind)
    GT = dram("GT", [128, 128, NTOK], BF16, skind)
    dbg = dram("dbg", [128, 4096], F32, "ExternalOutput") if debug else None

    from contextlib import ExitStack
    with ExitStack() as top:
        P = Prog(nc, top)
        sb = lambda ctx, name, shape, dt: ctx.enter_context(nc.sbuf_tensor(name, shape, dt))
        ps = [top.enter_context(nc.psum_tensor(f"ps{i}", [128, 512], F32)) for i in range(8)]
        vec_sb = sb(top, "vec_sb", [128, V_N], F32)
        icf_sb = sb(top, "icf_sb", [128, 64], F32)
        cst_sb = sb(top, "cst_sb", [128, C_N], F32)
        ones_bf = sb(top, "ones_bf", [128, 128], BF16)
        rstdB = sb(top, "rstdB", [128, NTOK], F32)
        P.add("sp", lambda e: e.dma_start(out=vec_sb[:], in_=vecs), writes=["vec"], dma="c0")
        P.add("sp", lambda e: e.dma_start(out=icf_sb[:], in_=icf), writes=["icf"], dma="c1")
        P.add("sp", lambda e: e.dma_start(out=cst_sb[:], in_=cst), writes=["cst"], dma="c2")
        P.add("pool", lambda e: e.memset(ones_bf[:], 1.0), writes=["ones"])

        def vcol(off, k):
            return vec_sb[:, off + k: off + k + 1]

        with ExitStack() as phA:
            hT = sb(phA, "hT", [128, KC, NCOL], BF16)
            ycat = sb(phA, "ycat", [128, KC, NTOK], BF16)
            rstd = sb(phA, "rstd", [128, NCOL], F32)
            rstd2 = sb(phA, "rstd2", [128, NCOL], F32)
            with ExitStack() as st:
                xin = [sb(st, f"xin{i}", [128, NCOL], F32) for i in range(2)]
                xsq = [sb(st, f"xsq{i}", [128, NCOL], BF16) for i in range(2)]
                sq = sb(st, "sqtmp", [128, NCOL], F32)
                for k in range(KC):
                    s = k % 2
                    P.add("sp", lambda e, k=k, s=s: e.dma_start(out=xin[s][:], in_=xT[k * 128:(k + 1) * 128, :]),
                          writes=[f"xin{s}"], dma=f"xin{s}")
                    P.add("dve", lambda e, k=k, s=s: e.tensor_scalar(out=hT[:, k, :], in0=xin[s][:], scalar1=vcol(V_G1, k),
                                                                     scalar2=None, op0=ALU.mult),
                          reads=[f"xin{s}", "vec"], writes=[f"hT{k}"])
                    P.add("act", lambda e, s=s: e.activation(out=xsq[s][:], in_=xin[s][:], func=AF.Square),
                          reads=[f"xin{s}"], writes=[f"xsq{s}"])
                    for cb in range(5):
                        c0 = cb * 512
                        n = min(512, NCOL - c0)
                        P.add("pe", lambda e, s=s, cb=cb, c0=c0, n=n, k=k: e.matmul(
                            ps[cb][:, 0:n], lhsT=ones_bf[:], rhs=xsq[s][:, c0:c0 + n], start=(k == 0), stop=(k == KC - 1)),
                              reads=[f"xsq{s}", "ones"], writes=[f"ps{cb}"])
                for cb in range(5):
                    c0 = cb * 512
                    n = min(512, NCOL - c0)
                    P.add("act", lambda e, cb=cb, c0=c0, n=n: e.activation(out=sq[:, c0:c0 + n], in_=ps[cb][:, 0:n], func=AF.Sqrt,
                                                                          scale=1.0 / D, bias=EPS),
                          reads=[f"ps{cb}"], writes=[f"sq{cb}"])
                    P.add("dve", lambda e, c0=c0, n=n: e.reciprocal(out=rstd[:, c0:c0 + n], in_=sq[:, c0:c0 + n]),
                          reads=[f"sq{cb}"], writes=[f"rstd{cb}"])
                    P.add("pool", lambda e, c0=c0, n=n: e.tensor_tensor(out=rstd2[:, c0:c0 + n], in0=rstd[:, c0:c0 + n],
                                                                        in1=rstd[:, c0:c0 + n], op=ALU.mult),
                          reads=[f"rstd{cb}"], writes=[f"rstd2{cb}"])
                P.flush()
            if stop_after == "A1a":
                return _finish(nc, P, dbg, [(rstd, 0, NCOL)])

            with ExitStack() as st:
                wA = [sb(st, f"wA{i}", [128, 3, KC, 128], BF16) for i in range(2)]
                cs = [sb(st, f"cs{i}", [128, 512], F32) for i in range(2)]
                cvb = [sb(st, f"cv{i}", [128, NCOL], F32) for i in range(2)]
                acc = [sb(st, f"acc{i}", [128, 512], F32) for i in range(2)]
                acc2 = [sb(st, f"accb{i}", [128, 512], F32) for i in range(2)]
                bank = 0
                it = 0
                def loadA(j):
                    s = j % 2
                    for w3, chunk in enumerate((j, 8 + j, 16 + j)):
                        P.add("pool", lambda e, s=s, w3=w3, chunk=chunk: e.dma_start(
                            out=wA[s][:, w3].rearrange("p k n -> p (k n)"), in_=w_in[chunk], max_dma_last_dim=4096),
                              writes=[f"wA{s}_{w3}"], dma=f"wA{s}_{w3}")
                loadA(0)
                for j in range(8):
                    s = j % 2
                    if j + 1 < 8:
                        loadA(j + 1)
                    for ti, (c0, n) in enumerate(TILES):
                        halo = (ti == 0)
                        bC, bV, bB = bank % 8, (bank + 1) % 8, (bank + 2) % 8
                        bank += 2 if halo else 3
                        for w3, bk in ((1, bC), (2, bV)) + (() if halo else ((0, bB),)):
                            for k in range(KC):
                                P.add("pe", lambda e, s=s, w3=w3, bk=bk, k=k, c0=c0, n=n: e.matmul(
                                    ps[bk][:, 0:n], lhsT=wA[s][:, w3, k, :], rhs=hT[:, k, c0:c0 + n], start=(k == 0), stop=(k == KC - 1)),
                                      reads=[f"wA{s}_{w3}", f"hT{k}"], writes=[f"ps{bk}"])
                        u = it % 2
                        it += 1
                        cvres = f"cv{s}_{ti}"
                        P.add("dve", lambda e, u=u, bC=bC, c0=c0, n=n: e.tensor_tensor(out=cs[u][:, 0:n], in0=ps[bC][:, 0:n],
                                                                                    in1=rstd2[:, c0:c0 + n], op=ALU.mult),
                              reads=[f"ps{bC}"], writes=[f"cs{u}"])
                        P.add("dve", lambda e, u=u, bV=bV, c0=c0, n=n, s=s: e.tensor_tensor(out=cvb[s][:, c0:c0 + n], in0=ps[bV][:, 0:n],
                                                                                         in1=cs[u][:, 0:n], op=ALU.mult),
                              reads=[f"ps{bV}", f"cs{u}"], writes=[cvres])
                        if halo:
                            continue
                        prev = f"cv{s}_{ti - 1}"
                        cw = lambda kk, j=j: vec_sb[:, V_CW + j * 3 + kk: V_CW + j * 3 + kk + 1]
                        P.add("pool", lambda e, u=u, s=s, c0=c0, n=n, j=j, cw=cw: e.tensor_scalar(
                            out=acc[u][:, 0:n], in0=cvb[s][:, c0:c0 + n], scalar1=cw(2), scalar2=vcol(V_CB, j), op0=ALU.mult, op1=ALU.add),
                              reads=[cvres, "vec"], writes=[f"acc{u}"])
                        P.add("dve", lambda e, u=u, s=s, c0=c0, n=n, cw=cw: e.scalar_tensor_tensor(
                            out=acc[u][:, 0:n], in0=cvb[s][:, c0 - 1:c0 - 1 + n], scalar=cw(1), in1=acc[u][:, 0:n], op0=ALU.mult, op1=ALU.add),
                              reads=[cvres, prev, f"acc{u}"], writes=[f"acc{u}"])
                        P.add("dve", lambda e, u=u, s=s, c0=c0, n=n, cw=cw: e.scalar_tensor_tensor(
                            out=acc[u][:, 0:n], in0=cvb[s][:, c0 - 2:c0 - 2 + n], scalar=cw(0), in1=acc[u][:, 0:n], op0=ALU.mult, op1=ALU.add),
                              reads=[cvres, prev, f"acc{u}"], writes=[f"acc{u}"])
                        P.add("pool", lambda e, u=u, c0=c0, n=n: e.tensor_tensor(out=acc2[u][:, 0:n], in0=acc[u][:, 0:n],
                                                                                in1=rstd[:, c0:c0 + n], op=ALU.mult),
                              reads=[f"acc{u}"], writes=[f"accb{u}"])
                        P.add("dve", lambda e, u=u, bB=bB, c0=c0, n=n, j=j: e.tensor_tensor(
                            out=ycat[:, j, c0 - HALO:c0 - HALO + n], in0=ps[bB][:, 0:n], in1=acc2[u][:, 0:n], op=ALU.mult),
                              reads=[f"ps{bB}", f"accb{u}"], writes=[f"ycat{j}_{ti}"])
                P.flush()
            if stop_after == "A1b":
                return _finish(nc, P, dbg, [(ycat[:, 0, :], 0, NTOK), (ycat[:, 7, :], 0, NTOK)])

            with ExitStack() as st:
                wP = [sb(st, f"wP{i}", [128, KC, 128], BF16) for i in range(2)]
                pw = sb(st, "pw", [128, 4, 2, 256], BF16)
                zb = [sb(st, f"z{i}", [128, NCOL], F32) for i in range(2)]
                sA = [sb(st, f"sA{i}", [128, NCOL], F32) for i in range(1)]
                sB = [sb(st, f"sB{i}", [128, NCOL], F32) for i in range(1)]
                pooled = [sb(st, f"pooled{i}", [128, NTOK], BF16) for i in range(2)]
                tmp16 = [sb(st, f"tmp16_{i}", [128, 16], F32) for i in range(2)]
                P.add("pool", lambda e: e.dma_start(out=pw[:].rearrange("p g c d -> p (g c d)"), in_=pool_w, max_dma_last_dim=4096),
                      writes=["pw"], dma="pw")
                bank = 0

                def loadP(j):
                    s = j % 2
                    P.add("pool", lambda e, s=s, j=j: e.dma_start(out=wP[s][:].rearrange("p k n -> p (k n)"), in_=w_in[24 + j],
                                                                  max_dma_last_dim=4096),
                          writes=[f"wP{s}"], dma=f"wP{s}")
                for g, win in enumerate((2, 4, 8, 16)):
                    for jj in range(2):
                        j = 2 * g + jj
                        s = j % 2
                        zi = jj
                        if j == 0:
                            loadP(0)
                        if j + 1 < 8:
                            loadP(j + 1)
                        for ti, (c0, n) in enumerate(TILES):
                            bk = bank % 8
                            bank += 1
                            for k in range(KC):
                                P.add("pe", lambda e, s=s, bk=bk, k=k, c0=c0, n=n: e.matmul(
                                    ps[bk][:, 0:n], lhsT=wP[s][:, k, :], rhs=hT[:, k, c0:c0 + n], start=(k == 0), stop=(k == KC - 1)),
                                      reads=[f"wP{s}", f"hT{k}"], writes=[f"ps{bk}"])
                            P.add("dve", lambda e, zi=zi, bk=bk, c0=c0, n=n: e.tensor_tensor(out=zb[zi][:, c0:c0 + n], in0=ps[bk][:, 0:n],
                                                                                          in1=rstd[:, c0:c0 + n], op=ALU.mult),
                                  reads=[f"ps{bk}"], writes=[f"z{zi}"])
                        z = zb[zi]
                        cur, cur_name, sh = z, f"z{zi}", 1
                        bufs = [(sA[0], "sA0"), (sB[0], "sB0")]
                        bi = 0
                        while sh < win:
                            dst, dname = bufs[bi % 2]
                            bi += 1
                            lo = 2 * sh - 1
                            P.add("dve", lambda e, dst=dst, cur=cur, lo=lo, sh=sh: e.tensor_tensor(
                                out=dst[:, lo:NCOL], in0=cur[:, lo:NCOL], in1=cur[:, lo - sh:NCOL - sh], op=ALU.add),
                                  reads=[cur_name], writes=[dname])
                            cur, cur_name = dst, dname
                            sh *= 2
                        pi = jj
                        P.add("dve", lambda e, pi=pi, cur=cur, z=z, win=win: e.scalar_tensor_tensor(
                            out=pooled[pi][:, 16:NTOK], in0=cur[:, HALO + 16:NCOL], scalar=1.0 / win, in1=z[:, HALO + 16:NCOL],
                            op0=ALU.mult, op1=ALU.subtract),
                              reads=[cur_name, f"z{zi}"], writes=[f"pooled{pi}"])
                        P.add("dve", lambda e, jj=jj, cur=cur, g=g: e.tensor_tensor(
                            out=tmp16[jj][:], in0=cur[:, HALO:HALO + 16], in1=icf_sb[:, g * 16:(g + 1) * 16], op=ALU.mult),
                              reads=[cur_name, "icf"], writes=[f"tmp16_{jj}"])
                        P.add("dve", lambda e, jj=jj, pi=pi, z=z: e.tensor_tensor(
                            out=pooled[pi][:, 0:16], in0=tmp16[jj][:], in1=z[:, HALO:HALO + 16], op=ALU.subtract),
                              reads=[f"tmp16_{jj}", f"z{zi}"], writes=[f"pooledh{pi}"])
                    for ti in range(4):
                        c0 = ti * 512
                        for dout in range(2):
                            bk = bank % 8
                            bank += 1
                            for cc in range(2):
                                pi = cc
                                P.add("pe", lambda e, g=g, cc=cc, dout=dout, bk=bk, pi=pi, c0=c0: e.matmul(
                                    ps[bk][:, 0:512], lhsT=pw[:, g, cc, dout * 128:(dout + 1) * 128], rhs=pooled[pi][:, c0:c0 + 512],
                                    start=(cc == 0), stop=(cc == 1)),
                                      reads=["pw", f"pooled{pi}", f"pooledh{pi}"], writes=[f"ps{bk}"])
                            jo = 8 + 2 * g + dout
                            P.add("dve", lambda e, bk=bk, jo=jo, c0=c0, g=g, dout=dout: e.tensor_scalar(
                                out=ycat[:, jo, c0:c0 + 512], in0=ps[bk][:, 0:512], scalar1=vcol(V_PS, 2 * g + dout), scalar2=None, op0=ALU.mult),
                                  reads=[f"ps{bk}", "vec"], writes=[f"ycat{jo}_{ti}"])
                P.flush()
            if stop_after == "A1c":
                return _finish(nc, P, dbg, [(ycat[:, 8, :], 0, NTOK), (ycat[:, 15, :], 0, NTOK)])

            with ExitStack() as st:
                wO = [sb(st, f"wO{i}", [128, KC, 128], BF16) for i in range(2)]
                xres = [sb(st, f"xres{i}", [128, NTOK], F32) for i in range(2)]
                x1sq = [sb(st, f"x1sq{i}", [128, 512], BF16) for i in range(4)]
                pending = [None]
                sq2 = sb(st, "sq2", [128, NTOK], F32)
                bank = 0
                it = 0
                def loadO(m):
                    s = m % 2
                    P.add("pool", lambda e, s=s, m=m: e.dma_start(out=wO[s][:].rearrange("p k n -> p (k n)"), in_=w_out[m], max_dma_last_dim=4096),
                          writes=[f"wO{s}"], dma=f"wO{s}")
                loadO(0)
                for m in range(KC):
                    s = m % 2
                    if m + 1 < KC:
                        loadO(m + 1)
                    xr4 = [f"xres{s}_{ti}" for ti in range(4)]
                    P.add("sp", lambda e, s=s, m=m: e.dma_start(out=xres[s][:], in_=xT[m * 128:(m + 1) * 128, HALO:NCOL]),
                          writes=xr4, dma=f"xres{s}")
                    for ti in range(4):
                        c0 = ti * 512
                        bk = bank % 4
                        bank += 1
                        for k in range(KC):
                            P.add("pe", lambda e, s=s, bk=bk, k=k, c0=c0: e.matmul(
                                ps[bk][:, 0:512], lhsT=wO[s][:, k, :], rhs=ycat[:, k, c0:c0 + 512], start=(k == 0), stop=(k == KC - 1)),
                                  reads=[f"wO{s}"], writes=[f"ps{bk}"])
                        if pending[0] is not None:
                            pending[0]()
                            pending[0] = None
                        u = it % 4
                        it += 1
                        P.add("dve", lambda e, s=s, bk=bk, c0=c0: e.tensor_tensor(out=xres[s][:, c0:c0 + 512], in0=ps[bk][:, 0:512],
                                                                                 in1=xres[s][:, c0:c0 + 512], op=ALU.add),
                              reads=[f"ps{bk}", xr4[ti]], writes=[xr4[ti]])
                        P.add("act", lambda e, s=s, u=u, c0=c0: e.activation(out=x1sq[u][:], in_=xres[s][:, c0:c0 + 512], func=AF.Square),
                              reads=[xr4[ti]], writes=[f"x1sq{u}"])

                        def _ones(u=u, ti=ti, m=m):
                            P.add("pe", lambda e: e.matmul(ps[4 + ti][:, 0:512], lhsT=ones_bf[:], rhs=x1sq[u][:],
                                                           start=(m == 0), stop=(m == KC - 1)),
                                  reads=[f"x1sq{u}", "ones"], writes=[f"ps{4 + ti}"])
                        pending[0] = _ones
                    P.add("sp", lambda e, s=s, m=m: e.dma_start(out=x1T[m * 128:(m + 1) * 128, :], in_=xres[s][:]),
                          reads=xr4, writes=["x1T"], dma=f"x1st{s}")
                if pending[0] is not None:
                    pending[0]()
                    pending[0] = None
                for ti in range(4):
                    c0 = ti * 512
                    P.add("act", lambda e, ti=ti, c0=c0: e.activation(out=sq2[:, c0:c0 + 512], in_=ps[4 + ti][:, 0:512], func=AF.Sqrt,
                                                                      scale=1.0 / D, bias=EPS),
                          reads=[f"ps{4 + ti}"], writes=[f"sq2_{ti}"])
                    P.add("dve", lambda e, c0=c0: e.reciprocal(out=rstdB[:, c0:c0 + 512], in_=sq2[:, c0:c0 + 512]),
                          reads=[f"sq2_{ti}"], writes=[f"rstdB{ti}"])
                P.flush()
        if stop_after == "A2":
            return _finish(nc, P, dbg, [(rstdB, 0, NTOK)])

        with ExitStack() as phG:
            sv = sb(phG, "sv", [128, 16, 16, 16], F32)
            si = sb(phG, "si", [128, 16, 16, 16], U32)
            with ExitStack() as st:
                h2T = sb(st, "h2T", [128, KC, NTOK], BF16)
                xr = [sb(st, f"xr{i}", [128, NTOK], F32) for i in range(2)]
                wQ = [sb(st, f"wQ{i}", [128, KC, 128], BF16) for i in range(2)]
                qT = [sb(st, f"qT{i}", [128, NTOK], BF16) for i in range(2)]
                keys_bf = sb(st, "keys_bf", [128, 2048], BF16)
                work = [sb(st, f"work{i}", [128, 128], F32) for i in range(16)]
                P.add("pool", lambda e: e.dma_start(out=keys_bf[:], in_=keysT, max_dma_last_dim=4096), writes=["keys"], dma="keys")
                for m in range(KC):
                    s = m % 2
                    P.add("sp", lambda e, s=s, m=m: e.dma_start(out=xr[s][:], in_=x1T[m * 128:(m + 1) * 128, :]),
                          writes=[f"xr{s}"], dma=f"xr{s}")
                    P.add("dve", lambda e, s=s, m=m: e.scalar_tensor_tensor(out=h2T[:, m, :], in0=xr[s][:], scalar=vcol(V_G2, m),
                                                                          in1=rstdB[:], op0=ALU.mult, op1=ALU.mult),
                          reads=[f"xr{s}", "vec"], writes=[f"h2T{m}"])
                    P.add("sp", lambda e, m=m: e.dma_start(out=h2T_d[m], in_=h2T[:, m, :]), reads=[f"h2T{m}"], writes=["h2T_d"],
                          dma=f"h2st{s}")
                bank = 0
                wi = 0
                def loadQ(hp):
                    s = hp % 2
                    P.add("pool", lambda e, s=s, hp=hp: e.dma_start(out=wQ[s][:].rearrange("p k n -> p (k n)"), in_=w_q[hp], max_dma_last_dim=4096),
                          writes=[f"wQ{s}"], dma=f"wQ{s}")
                loadQ(0)
                for hp in range(16):
                    s = hp % 2
                    if hp + 1 < 16:
                        loadQ(hp + 1)
                    if hp == 0:
                        for k in range(KC):
                            for ti in range(4):
                                P.add("pe", lambda e, s=s, k=k, ti=ti: e.matmul(
                                    ps[ti][:, 0:512], lhsT=wQ[s][:, k, :], rhs=h2T[:, k, ti * 512:(ti + 1) * 512], start=(k == 0), stop=(k == KC - 1)),
                                      reads=[f"wQ{s}", f"h2T{k}"], writes=[f"ps{ti}"])
                        for ti in range(4):
                            P.add("act", lambda e, s=s, ti=ti: e.activation(out=qT[s][:, ti * 512:(ti + 1) * 512], in_=ps[ti][:, 0:512], func=AF.Copy),
                                  reads=[f"ps{ti}"], writes=[f"qT{s}_{ti}"])
                    for ti in (range(4) if hp > 0 else ()):
                        c0 = ti * 512
                        bk = bank % 4
                        bank += 1
                        for k in range(KC):
                            P.add("pe", lambda e, s=s, bk=bk, k=k, c0=c0: e.matmul(
                                ps[bk][:, 0:512], lhsT=wQ[s][:, k, :], rhs=h2T[:, k, c0:c0 + 512], start=(k == 0), stop=(k == KC - 1)),
                                  reads=[f"wQ{s}", f"h2T{k}"], writes=[f"ps{bk}"])
                        P.add("act", lambda e, s=s, bk=bk, c0=c0: e.activation(out=qT[s][:, c0:c0 + 512], in_=ps[bk][:, 0:512], func=AF.Copy),
                              reads=[f"ps{bk}"], writes=[f"qT{s}_{ti}"])
                    for stl in range(16):
                        bk = 4 + stl // 4
                        cc = (stl % 4) * 128
                        P.add("pe", lambda e, s=s, stl=stl, bk=bk, cc=cc, hp=hp: e.matmul(
                            ps[bk][:, cc:cc + 128], lhsT=qT[s][:, stl * 128:(stl + 1) * 128], rhs=keys_bf[:, hp * 128:(hp + 1) * 128],
                            start=True, stop=True),
                              reads=[f"qT{s}_{stl // 4}", "keys"], writes=[f"ps{bk}"])
                    scrs = [ps[4 + stl // 4][:, (stl % 4) * 128:(stl % 4) * 128 + 128] for stl in range(16)]
                    bks = [4 + stl // 4 for stl in range(16)]
                    for half in (range(0, 8), range(8, 16)):
                        for stl in half:
                            P.add("dve", lambda e, scr=scrs[stl], stl=stl, hp=hp: e.max(out=sv[:, stl, hp, 0:8], in_=scr),
                                  reads=[f"ps{bks[stl]}"], writes=[f"svA{stl}"])
                        for stl in half:
                            P.add("dve", lambda e, scr=scrs[stl], stl=stl, hp=hp: e.match_replace(out=work[stl][:], in_to_replace=sv[:, stl, hp, 0:8],
                                                                                             in_values=scr, imm_value=NEG),
                                  reads=[f"ps{bks[stl]}", f"svA{stl}"], writes=[f"work{stl}"])
                        for stl in half:
                            P.add("dve", lambda e, scr=scrs[stl], stl=stl, hp=hp: e.max_index(out=si[:, stl, hp, 0:8], in_max=sv[:, stl, hp, 0:8], in_values=scr),
                                  reads=[f"ps{bks[stl]}", f"svA{stl}"], writes=[f"siA{stl}"])
                        for stl in half:
                            P.add("dve", lambda e, stl=stl, hp=hp: e.max(out=sv[:, stl, hp, 8:16], in_=work[stl][:]),
                                  reads=[f"work{stl}"], writes=[f"svB{stl}"])
                        for stl in half:
                            P.add("dve", lambda e, stl=stl, hp=hp: e.max_index(out=si[:, stl, hp, 8:16], in_max=sv[:, stl, hp, 8:16], in_values=work[stl][:]),
                                  reads=[f"work{stl}", f"svB{stl}"], writes=[f"siB{stl}"])
                P.flush()
            if stop_after == "A3a":
                return _finish(nc, P, dbg, [(sv[:, 0].rearrange("p a b -> p (a b)"), 0, 256),
                                            (si[:, 0].rearrange("p a b -> p (a b)"), 0, 256),
                                            (sv[:, 15].rearrange("p a b -> p (a b)"), 0, 256),
                                            (si[:, 15].rearrange("p a b -> p (a b)"), 0, 256)])
            with ExitStack() as st:
                cand = sb(st, "cand", [128, 8, 16, 16], F32)
                work2 = [sb(st, f"work2_{i}", [128, 256], F32) for i in range(4)]
                top = [sb(st, f"top{i}", [128, 8, 16], F32) for i in range(2)]
                pos = [sb(st, f"pos{i}", [128, 8, 16], U32) for i in range(2)]
                dsub = sb(st, "dsub", [128, 8, 16], F32)
                ex = sb(st, "ex", [128, 8, 16], F32)
                zsum = sb(st, "zsum", [128, 8], F32)
                rz = sb(st, "rz", [128, 8], F32)
                gt = sb(st, "gt", [128, 8, 16], F32)
                a_u = sb(st, "a_u", [128, 8, 16], U32)
                b_u = sb(st, "b_u", [128, 8, 16], U32)
                a_f = sb(st, "a_f", [128, 8, 16], F32)
                b_f = sb(st, "b_f", [128, 8, 16], F32)
                si_f = sb(st, "si_f", [128, 8, 2, 16], F32)
                eq = sb(st, "eq", [128, 8, 16, 16], F32)
                sel = sb(st, "sel", [128, 8, 16, 16], F32)
                I_f = sb(st, "I_f", [128, 8, 16], F32)
                J_f = sb(st, "J_f", [128, 8, 16], F32)
                IT = [sb(st, f"IT{i}", [128, 128], BF16) for i in range(2)]
                JT = [sb(st, f"JT{i}", [128, 128], BF16) for i in range(2)]
                gT = [sb(st, f"gT{i}", [128, 128], BF16) for i in range(2)]
                iota_bf = sb(st, "iota_bf", [128, 128], BF16)
                Pm = [sb(st, f"Pm{i}", [128, 128, 32], BF16) for i in range(3)]
                Qm = [sb(st, f"Qm{i}", [128, 128, 32], BF16) for i in range(3)]
                iota3 = sb(st, "iota3", [128, 128, 32], BF16)
                Gsb = [sb(st, f"Gsb{i}", [128, 128, 128], BF16) for i in range(2)]
                ident = cst_sb[:, C_ID:C_ID + 128]
                iota16 = cst_sb[:, C_I16:C_I16 + 16]
                P.add("dve", lambda e: e.tensor_copy(out=iota_bf[:], in_=cst_sb[:, C_I128:C_I128 + 128]), reads=["cst"], writes=["iota_bf"])
                P.add("dve", lambda e: e.tensor_copy(out=iota3[:], in_=iota_bf[:].unsqueeze(2).broadcast_to([128, 128, 32])),
                      reads=["iota_bf"], writes=["iota3"])
                cntG = {'b': 0, 'h': 0}
                def front(stl):
                    u2 = stl % 2
                    svs = sv[:, stl].rearrange("p (h two) k -> p h two k", two=2)
                    sis = si[:, stl].rearrange("p (h two) k -> p h two k", two=2)
                    P.add("pool", lambda e, svs=svs: e.tensor_tensor(
                        out=cand[:], in0=svs[:, :, 0, :].unsqueeze(3).broadcast_to([128, 8, 16, 16]),
                        in1=svs[:, :, 1, :].unsqueeze(2).broadcast_to([128, 8, 16, 16]), op=ALU.add),
                          reads=[], writes=["cand"])
                    chs = [cand[:, h].rearrange("p a b -> p (a b)") for h in range(8)]
                    for hh in (range(0, 4), range(4, 8)):
                        for h in hh:
                            P.add("dve", lambda e, ch=chs[h], h=h, u2=u2: e.max(out=top[u2][:, h, 0:8], in_=ch), reads=["cand"], writes=[f"tA{h}"])
                        for h in hh:
                            P.add("dve", lambda e, ch=chs[h], h=h, u2=u2: e.match_replace(out=work2[h % 4][:], in_to_replace=top[u2][:, h, 0:8], in_values=ch,
                                                                                     imm_value=NEG), reads=["cand", f"tA{h}"], writes=[f"w2_{h % 4}"])
                        for h in hh:
                            P.add("dve", lambda e, ch=chs[h], h=h, u2=u2: e.max_index(out=pos[u2][:, h, 0:8], in_max=top[u2][:, h, 0:8], in_values=ch),
                                  reads=["cand", f"tA{h}"], writes=[f"pA{h}"])
                        for h in hh:
                            P.add("dve", lambda e, h=h, u2=u2: e.max(out=top[u2][:, h, 8:16], in_=work2[h % 4][:]), reads=[f"w2_{h % 4}"], writes=[f"tB{h}"])
                        for h in hh:
                            P.add("dve", lambda e, h=h, u2=u2: e.max_index(out=pos[u2][:, h, 8:16], in_max=top[u2][:, h, 8:16], in_values=work2[h % 4][:]),
                                  reads=[f"w2_{h % 4}", f"tB{h}"], writes=[f"pB{h}"])
                        yield
                    tall = [f"tA{h}" for h in range(8)] + [f"tB{h}" for h in range(8)]
                    pall = [f"pA{h}" for h in range(8)] + [f"pB{h}" for h in range(8)]
                    P.add("pool", lambda e, u2=u2: e.tensor_tensor(out=dsub[:], in0=top[u2][:], in1=top[u2][:, :, 0:1].broadcast_to([128, 8, 16]),
                                                                  op=ALU.subtract), reads=tall, writes=["dsub"])
                    P.add("act", lambda e: e.activation(out=ex[:], in_=dsub[:], func=AF.Exp), reads=["dsub"], writes=["ex"])
                    P.add("dve", lambda e: e.tensor_reduce(out=zsum[:], in_=ex[:], axis=AX.X, op=ALU.add), reads=["ex"], writes=["zsum"])
                    P.add("dve", lambda e: e.reciprocal(out=rz[:], in_=zsum[:]), reads=["zsum"], writes=["rz"])
                    P.add("pool", lambda e: e.tensor_tensor(out=gt[:], in0=ex[:], in1=rz[:].unsqueeze(2).broadcast_to([128, 8, 16]), op=ALU.mult),
                          reads=["ex", "rz"], writes=["gt"])
                    P.add("dve", lambda e, u2=u2: e.tensor_single_scalar(out=a_u[:], in_=pos[u2][:], scalar=4, op=ALU.logical_shift_right),
                          reads=pall, writes=["a_u"])
                    P.add("dve", lambda e, u2=u2: e.tensor_single_scalar(out=b_u[:], in_=pos[u2][:], scalar=15, op=ALU.bitwise_and),
                          reads=pall, writes=["b_u"])
                    P.add("dve", lambda e: e.tensor_copy(out=a_f[:], in_=a_u[:]), reads=["a_u"], writes=["a_f"])
                    P.add("dve", lambda e: e.tensor_copy(out=b_f[:], in_=b_u[:]), reads=["b_u"], writes=["b_f"])
                    P.add("dve", lambda e, sis=sis: e.tensor_copy(out=si_f[:], in_=sis), reads=[], writes=["si_f"])
                    dec = ((a_f, "a_f", I_f, "I_f", eq, "eq"), (b_f, "b_f", J_f, "J_f", cand, "cand"))
                    for which, (xf, xname, dst, dname, ebuf, ename) in enumerate(dec):
                        P.add("dve", lambda e, xf=xf, ebuf=ebuf: e.tensor_tensor(
                            out=ebuf[:], in0=xf[:].unsqueeze(3).broadcast_to([128, 8, 16, 16]),
                            in1=iota16.unsqueeze(1).unsqueeze(1).broadcast_to([128, 8, 16, 16]), op=ALU.is_equal),
                              reads=[xname, "cst"], writes=[ename])
                    for which, (xf, xname, dst, dname, ebuf, ename) in enumerate(dec):
                        P.add("pool", lambda e, which=which, ebuf=ebuf: e.tensor_tensor(
                            out=sel[:], in0=ebuf[:], in1=si_f[:, :, which, :].unsqueeze(2).broadcast_to([128, 8, 16, 16]), op=ALU.mult),
                              reads=[ename, "si_f"], writes=["sel"])
                        P.add("dve", lambda e, dst=dst: e.tensor_reduce(out=dst[:], in_=sel[:], axis=AX.X, op=ALU.add), reads=["sel"], writes=[dname])
                    yield
                    tb = stl % 2
                    for ci, (src, sname) in enumerate(((I_f, "I_f"), (J_f, "J_f"), (gt, "gt"))):
                        P.add("pe", lambda e, tb=tb, ci=ci, src=src: e.transpose(out=ps[tb][:, ci * 128:(ci + 1) * 128],
                                                                               in_=src[:].rearrange("p h k -> p (h k)"), identity=ident),
                              reads=[sname, "cst"], writes=[f"ps{tb}"])
                    for ci, (dst, dname) in enumerate(((IT[u2], f"IT{u2}"), (JT[u2], f"JT{u2}"), (gT[u2], f"gT{u2}"))):
                        P.add("act", lambda e, tb=tb, ci=ci, dst=dst: e.activation(out=dst[:], in_=ps[tb][:, ci * 128:(ci + 1) * 128], func=AF.Copy),
                              reads=[f"ps{tb}"], writes=[dname])
                def back(stl, gen):
                    u2 = stl % 2
                    g2 = stl % 2
                    for qtr in range(4):
                        r = cntG['h'] % 3
                        cntG['h'] += 1
                        t0 = qtr * 32
                        P.add("dve", lambda e, r=r, u2=u2, t0=t0: e.tensor_tensor(
                            out=Qm[r][:], in0=iota3[:], in1=JT[u2][:, t0:t0 + 32].unsqueeze(1).broadcast_to([128, 128, 32]), op=ALU.is_equal),
                              reads=[f"JT{u2}", "iota3"], writes=[f"Qm{r}"])
                        P.add("dve", lambda e, r=r, u2=u2, t0=t0: e.tensor_tensor(
                            out=Pm[r][:], in0=iota3[:], in1=IT[u2][:, t0:t0 + 32].unsqueeze(1).broadcast_to([128, 128, 32]), op=ALU.is_equal),
                              reads=[f"IT{u2}", "iota3"], writes=[f"Pm{r}"])
                        P.add("dve", lambda e, r=r, u2=u2, t0=t0: e.tensor_tensor(
                            out=Pm[r][:], in0=Pm[r][:], in1=gT[u2][:, t0:t0 + 32].unsqueeze(1).broadcast_to([128, 128, 32]), op=ALU.mult),
                              reads=[f"Pm{r}", f"gT{u2}"], writes=[f"Pm{r}"])
                        if gen is not None:
                            next(gen, None)
                        for q4 in range(8):
                            bk = 2 + cntG['b'] % 6
                            cntG['b'] += 1
                            for tt in range(4):
                                tl = q4 * 4 + tt
                                P.add("pe", lambda e, r=r, bk=bk, tt=tt, tl=tl: e.matmul(
                                    ps[bk][:, tt * 128:(tt + 1) * 128], lhsT=Pm[r][:, :, tl], rhs=Qm[r][:, :, tl], start=True, stop=True),
                                      reads=[f"Pm{r}", f"Qm{r}"], writes=[f"ps{bk}"])
                            tg = t0 + q4 * 4
                            dstv = Gsb[g2][:, :, tg:tg + 4]
                            srcv = ps[bk][:, 0:512].rearrange("p (t j) -> p j t", t=4)
                            P.add("act", lambda e, dstv=dstv, srcv=srcv: e.activation(out=dstv, in_=srcv, func=AF.Copy),
                                  reads=[f"ps{bk}"], writes=[f"Gsb{g2}_{qtr}_{q4}"])
                    allg = [f"Gsb{g2}_{qtr}_{q4}" for qtr in range(4) for q4 in range(8)]
                    for jb in range(8):
                        j0 = jb * 16
                        P.add("sp", lambda e, g2=g2, j0=j0, stl=stl: e.dma_start(
                            out=GT[j0:j0 + 16, :, stl * 128:(stl + 1) * 128].rearrange("j i t -> i j t"), in_=Gsb[g2][:, j0:j0 + 16, :]),
                              reads=allg, writes=["GT"], dma=f"gst{g2}_{jb % 2}")
                for _ in front(0):
                    pass
                for stl in range(16):
                    gen = front(stl + 1) if stl + 1 < 16 else None
                    back(stl, gen)
                    if gen is not None:
                        for _ in gen:
                            pass
                P.flush()
            if stop_after == "A3b":
                return _finish(nc, P, dbg, [(I_f[:].rearrange("p h k -> p (h k)"), 0, 128), (J_f[:].rearrange("p h k -> p (h k)"), 0, 128),
                                            (gt[:].rearrange("p h k -> p (h k)"), 0, 128)])
        GELU = AF.Gelu_apprx_tanh if GELU_TANH else AF.Gelu
        NTB = nt_pass // 512
        NG = 128 // grp
        with ExitStack() as phB:
            h2p = sb(phB, "h2p", [128, KC, nt_pass], BF16)
            accb = sb(phB, "accB", [128, KC, nt_pass], F32)
            UTb = [sb(phB, f"UTb{i}", [128, KC, 128], BF16) for i in range(3)]
            Vb = [[sb(phB, f"Vb{g}_{i}", [128, D], BF16) for i in range(grp)] for g in range(2)]
            Gc = [[sb(phB, f"Gc{g}_{i}", [128, nt_pass], BF16) for i in range(grp)] for g in range(2)]
            GA = [[sb(phB, f"GA{g}_{i}", [128, nt_pass], BF16) for i in range(grp)] for g in range(2)]
            gel = [sb(phB, f"gel{i}", [128, 512], BF16) for i in range(2)]
            xf = [sb(phB, f"xf{i}", [128, nt_pass], F32) for i in range(2)]
            ysq = [sb(phB, f"ysq{i}", [128, 512], BF16) for i in range(2)]
            sq3 = sb(phB, "sq3", [128, nt_pass], F32)
            r3 = sb(phB, "r3", [128, nt_pass], F32)
            cnt = {"a": 0, "b": 0, "g": 0, "u": 0}
            npass = NTOK // nt_pass
            if True:
                def load_h2p(tp0):
                    P.add("sp", lambda e, tp0=tp0: e.dma_start(out=h2p[:], in_=h2T_d[:, :, tp0:tp0 + nt_pass].rearrange("k p t -> p k t")),
                          writes=["h2p"], dma="h2p")

                def mm1(gi, tp0):
                    gs = gi % 2
                    for ci in range(grp):
                        c = gi * grp + ci
                        us = cnt["u"] % 3
                        cnt["u"] += 1
                        P.add("pool", lambda e, us=us, c=c: e.dma_start(out=UTb[us][:].rearrange("p k n -> p (k n)"), in_=UT[c], max_dma_last_dim=4096),
                              writes=[f"UTb{us}"], dma=f"UTb{us}")
                        P.add("pool", lambda e, gs=gs, ci=ci, c=c: e.dma_start(out=Vb[gs][ci][:], in_=VP[c], max_dma_last_dim=4096),
                              writes=[f"Vb{gs}_{ci}"], dma=f"Vb{gs}_{ci}")
                        P.add("sp", lambda e, gs=gs, ci=ci, c=c, tp0=tp0: e.dma_start(out=Gc[gs][ci][:], in_=GT[c, :, tp0:tp0 + nt_pass]),
                              writes=[f"Gc{gs}_{ci}"], dma=f"Gc{gs}_{ci}")
                        for tb in range(NTB):
                            a = cnt["a"] % 2
                            cnt["a"] += 1
                            for k in range(KC):
                                P.add("pe", lambda e, us=us, a=a, k=k, tb=tb: e.matmul(
                                    ps[a][:, 0:512], lhsT=UTb[us][:, k, :], rhs=h2p[:, k, tb * 512:(tb + 1) * 512], start=(k == 0), stop=(k == KC - 1)),
                                      reads=[f"UTb{us}", "h2p"], writes=[f"ps{a}"])
                            x = cnt["g"] % 2
                            cnt["g"] += 1
                            P.add("act", lambda e, a=a, x=x: e.activation(out=gel[x][:], in_=ps[a][:, 0:512], func=GELU),
                                  reads=[f"ps{a}"], writes=[f"gel{x}"])
                            P.add("dve", lambda e, gs=gs, ci=ci, x=x, tb=tb: e.tensor_tensor(
                                out=GA[gs][ci][:, tb * 512:(tb + 1) * 512], in0=gel[x][:], in1=Gc[gs][ci][:, tb * 512:(tb + 1) * 512], op=ALU.mult),
                                  reads=[f"gel{x}", f"Gc{gs}_{ci}"], writes=[f"GA{gs}_{ci}_{tb}"])

                def mm2(gi):
                    gs = gi % 2
                    for m in range(KC):
                        for tb in range(NTB):
                            b = 2 + cnt["b"] % 6
                            cnt["b"] += 1
                            for ci in range(grp):
                                P.add("pe", lambda e, gs=gs, ci=ci, m=m, tb=tb, b=b: e.matmul(
                                    ps[b][:, 0:512], lhsT=Vb[gs][ci][:, m * 128:(m + 1) * 128], rhs=GA[gs][ci][:, tb * 512:(tb + 1) * 512],
                                    start=(ci == 0), stop=(ci == grp - 1)),
                                      reads=[f"Vb{gs}_{ci}", f"GA{gs}_{ci}_{tb}"], writes=[f"ps{b}"])
                            dst = accb[:, m, tb * 512:(tb + 1) * 512]
                            if gi == 0:
                                P.add("act", lambda e, dst=dst, b=b: e.activation(out=dst, in_=ps[b][:, 0:512], func=AF.Copy),
                                      reads=[f"ps{b}"], writes=[f"acc{m}_{tb}"])
                            else:
                                P.add("dve", lambda e, dst=dst, b=b: e.tensor_tensor(out=dst, in0=ps[b][:, 0:512], in1=dst, op=ALU.add),
                                      reads=[f"ps{b}", f"acc{m}_{tb}"], writes=[f"acc{m}_{tb}"])

                def tail(tp0):
                  for m in range(KC):
                    s = m % 2
                    P.add("sp", lambda e, s=s, m=m, tp0=tp0: e.dma_start(out=xf[s][:], in_=x1T[m * 128:(m + 1) * 128, tp0:tp0 + nt_pass]),
                          writes=[f"xf{s}"], dma=f"xf{s}")
                    accs = [f"acc{m}_{tb}" for tb in range(NTB)]
                    P.add("dve", lambda e, s=s, m=m: e.tensor_tensor(out=accb[:, m, :], in0=accb[:, m, :], in1=xf[s][:], op=ALU.add),
                          reads=[f"xf{s}"] + accs, writes=accs)
                    for tb in range(NTB):
                        y = (m * NTB + tb) % 2
                        P.add("act", lambda e, y=y, m=m, tb=tb: e.activation(out=ysq[y][:], in_=accb[:, m, tb * 512:(tb + 1) * 512], func=AF.Square),
                              reads=[f"acc{m}_{tb}"], writes=[f"ysq{y}"])
                        P.add("pe", lambda e, y=y, m=m, tb=tb: e.matmul(ps[6 + tb][:, 0:512], lhsT=ones_bf[:], rhs=ysq[y][:],
                                                                       start=(m == 0), stop=(m == KC - 1)),
                              reads=[f"ysq{y}", "ones"], writes=[f"ps{6 + tb}"])
                  for tb in range(NTB):
                    P.add("act", lambda e, tb=tb: e.activation(out=sq3[:, tb * 512:(tb + 1) * 512], in_=ps[6 + tb][:, 0:512], func=AF.Sqrt,
                                                               scale=1.0 / D, bias=EPS), reads=[f"ps{6 + tb}"], writes=[f"sq3_{tb}"])
                    P.add("dve", lambda e, tb=tb: e.reciprocal(out=r3[:, tb * 512:(tb + 1) * 512], in_=sq3[:, tb * 512:(tb + 1) * 512]),
                          reads=[f"sq3_{tb}"], writes=[f"r3_{tb}"])
                  for m in range(KC):
                    accs = [f"acc{m}_{tb}" for tb in range(NTB)]
                    P.add("dve", lambda e, m=m: e.scalar_tensor_tensor(out=accb[:, m, :], in0=accb[:, m, :], scalar=vcol(V_G3, m), in1=r3[:],
                                                                      op0=ALU.mult, op1=ALU.mult),
                          reads=accs + [f"r3_{tb}" for tb in range(NTB)] + ["vec"], writes=accs)
                    P.add("sp", lambda e, m=m, tp0=tp0: e.dma_start(out=outT[m * 128:(m + 1) * 128, tp0:tp0 + nt_pass], in_=accb[:, m, :]),
                          reads=accs, writes=["outT"], dma=f"ost{m % 2}")

                load_h2p(0)
                mm1(0, 0)
                for pi in range(npass):
                    tp0 = pi * nt_pass
                    for gi in range(NG):
                        if gi + 1 < NG:
                            mm1(gi + 1, tp0)
                        elif pi + 1 < npass:
                            load_h2p(tp0 + nt_pass)
                            mm1(0, tp0 + nt_pass)
                        mm2(gi)
                    tail(tp0)
                P.flush()
        return _finish(nc, P, dbg, [])


def _finish(nc, P, dbg, taps):
    if dbg is not None:
        off = 0
        for i, (ap, c0, n) in enumerate(taps):
            src = ap[:, c0:c0 + n]
            P.add("pool", lambda e, src=src, off=off, n=n: e.dma_start(out=dbg[:, off:off + n], in_=src, max_dma_last_dim=2048),
                  writes=[f"dbg{i}"], dma=f"dbg{i}")
            off += n
    P.flush()
    return nc


def _host_layout(inputs):
    f = lambda a: np.ascontiguousarray(a, dtype=np.float32)
    x = inputs["x"]
    shared = {}
    wi = inputs["w_in"][0]
    shared["w_in"] = f(wi.reshape(16, 128, 32, 128).transpose(2, 1, 0, 3).reshape(32, 128, 2048))
    wo = inputs["w_out"][0]
    shared["w_out"] = f(wo.reshape(16, 128, 16, 128).transpose(2, 1, 0, 3).reshape(16, 128, 2048))
    wq = inputs["peer_w_q"][0]
    shared["w_q"] = f(wq.reshape(16, 128, 16, 128).transpose(2, 1, 0, 3).reshape(16, 128, 2048))
    pw = inputs["pool_w"][0]
    shared["pool_w"] = f(pw.reshape(4, 2, 128, 256).transpose(2, 0, 1, 3).reshape(128, 2048))
    sk = inputs["peer_sub_keys"][0]
    shared["keysT"] = f(sk.reshape(16, 128, 128).transpose(2, 0, 1).reshape(128, 2048))
    vecs = np.zeros((128, V_N), np.float32)
    vecs[:, V_G1:V_G1 + 16] = inputs["norm_mix"][0].reshape(16, 128).T
    vecs[:, V_G2:V_G2 + 16] = inputs["norm_ffn"][0].reshape(16, 128).T
    vecs[:, V_G3:V_G3 + 16] = inputs["norm_final"].reshape(16, 128).T
    vecs[:, V_CW:V_CW + 24] = inputs["conv_w"][0].reshape(3, 8, 128).transpose(2, 1, 0).reshape(128, 24)
    vecs[:, V_CB:V_CB + 8] = inputs["conv_b"][0].reshape(8, 128).T
    vecs[:, V_PS:V_PS + 8] = inputs["pool_scale"][0].reshape(8, 128).T
    shared["vecs"] = vecs
    cst = np.zeros((128, C_N), np.float32)
    cst[:, C_ID:C_ID + 128] = np.eye(128, dtype=np.float32)
    cst[:, C_I128:C_I128 + 128] = np.arange(128, dtype=np.float32)[None, :]
    cst[:, C_I16:C_I16 + 16] = np.arange(16, dtype=np.float32)[None, :]
    shared["cst"] = cst
    u = inputs["peer_u"][0]
    shared["UT"] = f(u.reshape(128, 128, 16, 128).transpose(1, 3, 2, 0).reshape(128, 128, 2048))
    v = inputs["peer_v"][0]
    shared["VP"] = f(v.reshape(128, 128, 2048).transpose(1, 0, 2))
    in_maps = []
    for c in range(8):
        b, s0 = c // 4, (c % 4) * NTOK
        xs = np.zeros((NCOL, D), np.float32)
        if s0 == 0:
            xs[HALO:] = x[b, :NTOK]
        else:
            xs[:] = x[b, s0 - HALO:s0 + NTOK]
        m = dict(shared)
        m["xT"] = f(xs.T)
        ic = np.zeros((4, 16), np.float32)
        for g, w in enumerate((2, 4, 8, 16)):
            pos = np.arange(s0 + 1, s0 + 17, dtype=np.float32)
            ic[g] = 1.0 / np.minimum(pos, float(w))
        m["icf"] = f(np.broadcast_to(ic.reshape(1, 64), (128, 64)))
        in_maps.append(m)
    return in_maps


def kernel(**inputs):
    in_maps = _host_layout(inputs)
    nc = build_nc()
    res = run_bass_kernel_spmd(nc, in_maps, core_ids=list(range(8)))
    out = np.zeros((2, 8192, D), np.float32)
    for c in range(8):
        b, s0 = c // 4, (c % 4) * NTOK
        out[b, s0:s0 + NTOK] = res.results[c]["outT"].T
    return out
```
